# Optimizing a Trainium2 kernel written in Bass

```python
import math
import jax
import jax.numpy as jnp
from jax import lax
import numpy as np

D_MODEL = 4096
BATCH = 1
SEQ = 8192
DEPTH = 1

HEAD_DIM = 128
MIX_WIDTH = D_MODEL
ATTN_HEADS = MIX_WIDTH // 2 // HEAD_DIM
ATTN_WIDTH = ATTN_HEADS * HEAD_DIM
CONV_WIDTH = MIX_WIDTH - ATTN_WIDTH
CONV_GROUPS = CONV_WIDTH // HEAD_DIM
CONV_KERNEL = 31
Q_BLOCK = 128
FORGET_BIAS_MEAN = 2.0
IN_WIDTH = 3 * ATTN_WIDTH + ATTN_HEADS + 2 * CONV_WIDTH

N_EXPERTS = 64
TOP_K = 6
EXPERT_FF = D_MODEL // 8
SHARED_FF = EXPERT_FF
N_GROUPS = 8
TOPK_GROUPS = 4
ROUTE_SCALE = 2.5
MOE_BLOCK = 128

LN_EPS = 1e-5

kernel_name = "hybrid_fox_conformer_moe_deepnorm_adaln"


def layer_norm(x, g, b):
    xf = x.astype(jnp.float32)
    mu = jnp.mean(xf, axis=-1, keepdims=True)
    var = jnp.mean(jnp.square(xf - mu), axis=-1, keepdims=True)
    return ((xf - mu) * lax.rsqrt(var + LN_EPS)).astype(x.dtype) * g + b


def forgetting_attention(q, k, v, log_f):
    B, S, H, Dh = q.shape
    scale = Dh ** -0.5
    cum = jnp.transpose(jnp.cumsum(log_f, axis=1), (0, 2, 1))
    k_pos = jnp.arange(S)

    def block(i):
        start = i * Q_BLOCK
        qb = lax.dynamic_slice_in_dim(q, start, Q_BLOCK, axis=1)
        cq = lax.dynamic_slice_in_dim(cum, start, Q_BLOCK, axis=2)
        s = jnp.einsum('bqhd,bkhd->bhqk', qb, k).astype(jnp.float32) * scale
        s = s + cq[..., None] - cum[:, :, None, :]
        q_pos = start + jnp.arange(Q_BLOCK)
        s = jnp.where(k_pos[None, :] <= q_pos[:, None], s, -jnp.inf)
        p = jax.nn.softmax(s, axis=-1)
        return jnp.einsum('bhqk,bkhd->bqhd', p.astype(v.dtype), v)

    out = lax.map(block, jnp.arange(S // Q_BLOCK))
    return jnp.moveaxis(out, 0, 1).reshape(B, S, H * Dh)


def conformer_conv(val, gate, w_dw, b_dw, norm_g, norm_b):
    B, S, C = val.shape
    u = val * jax.nn.sigmoid(gate)
    u = lax.conv_general_dilated(
        u, w_dw[:, None, :], window_strides=(1,), padding=[(CONV_KERNEL - 1, 0)],
        dimension_numbers=('NWC', 'WIO', 'NWC'), feature_group_count=C) + b_dw
    ug = u.reshape(B, S, CONV_GROUPS, C // CONV_GROUPS).astype(jnp.float32)
    mu = jnp.mean(ug, axis=-1, keepdims=True)
    var = jnp.mean(jnp.square(ug - mu), axis=-1, keepdims=True)
    u = ((ug - mu) * lax.rsqrt(var + LN_EPS)).reshape(B, S, C).astype(val.dtype) * norm_g + norm_b
    return jax.nn.silu(u)


def moe_ffn(h, w_router, router_bias, w_exp_gate, w_exp_up, w_exp_down, w_sh_gate, w_sh_up, w_sh_down):
    B, S, D = h.shape
    T = B * S
    xf = h.reshape(T, D)
    scores = jax.nn.sigmoid((xf @ w_router).astype(jnp.float32))
    biased = scores + router_bias.astype(jnp.float32)
    per_group = N_EXPERTS // N_GROUPS
    grp_score = lax.top_k(biased.reshape(T, N_GROUPS, per_group), 2)[0].sum(-1)
    _, grp_idx = lax.top_k(grp_score, TOPK_GROUPS)
    grp_mask = jax.nn.one_hot(grp_idx, N_GROUPS, dtype=jnp.float32).sum(1) > 0
    exp_mask = jnp.repeat(grp_mask, per_group, axis=1)
    _, top_idx = lax.top_k(jnp.where(exp_mask, biased, -jnp.inf), TOP_K)
    top_w = jnp.take_along_axis(scores, top_idx, axis=1)
    top_w = top_w / jnp.sum(top_w, axis=-1, keepdims=True) * ROUTE_SCALE

    TK = T * TOP_K
    expert_flat = top_idx.reshape(TK).astype(jnp.int32)
    token_flat = jnp.repeat(jnp.arange(T, dtype=jnp.int32), TOP_K)
    gate_flat = top_w.reshape(TK).astype(xf.dtype)
    order = jnp.argsort(expert_flat)
    sorted_e = expert_flat[order]
    sorted_tok = token_flat[order]
    sorted_g = gate_flat[order]
    counts = jnp.bincount(expert_flat, length=N_EXPERTS).astype(jnp.int32)
    group_start = jnp.cumsum(counts) - counts
    padded = (counts + MOE_BLOCK - 1) // MOE_BLOCK * MOE_BLOCK
    padded_end = jnp.cumsum(padded)
    padded_start = padded_end - padded
    dest = padded_start[sorted_e] + (jnp.arange(TK, dtype=jnp.int32) - group_start[sorted_e])
    n_blocks = (TK + N_EXPERTS * (MOE_BLOCK - 1) + MOE_BLOCK - 1) // MOE_BLOCK
    n_rows = n_blocks * MOE_BLOCK
    row_tok = jnp.zeros((n_rows,), jnp.int32).at[dest].set(sorted_tok)
    row_gate = jnp.zeros((n_rows,), xf.dtype).at[dest].set(sorted_g)
    block_start = jnp.arange(n_blocks, dtype=jnp.int32) * MOE_BLOCK
    block_e = jnp.clip(jnp.searchsorted(padded_end, block_start, side='right'), 0, N_EXPERTS - 1)

    def body(acc, blk):
        tok, g, e = blk
        xb = xf[tok]
        hid = jax.nn.silu(xb @ w_exp_gate[e]) * (xb @ w_exp_up[e])
        y = hid @ w_exp_down[e]
        return acc.at[tok].add(y * g[:, None]), None

    routed, _ = lax.scan(body, jnp.zeros_like(xf),
                         (row_tok.reshape(n_blocks, MOE_BLOCK), row_gate.reshape(n_blocks, MOE_BLOCK), block_e))
    shared = (jax.nn.silu(xf @ w_sh_gate) * (xf @ w_sh_up)) @ w_sh_down
    return (routed + shared).reshape(B, S, D)


def setup_inputs(seed: int = 0) -> dict:
    key = jax.random.key(seed)
    ks = jax.random.split(key, 23)
    L, D, E, F = DEPTH, D_MODEL, N_EXPERTS, EXPERT_FF
    beta = (8.0 * DEPTH) ** -0.25
    nrm = jax.random.normal
    return {
        'x': nrm(ks[0], (BATCH, SEQ, D)),
        'c': nrm(ks[1], (BATCH, D)),
        'w_ada': nrm(ks[2], (L, D, 6 * D)) * (0.5 * D ** -0.5),
        'b_ada': 0.02 * nrm(ks[3], (L, 6 * D)),
        'w_in': nrm(ks[4], (L, D, IN_WIDTH)) * D ** -0.5,
        'b_forget': FORGET_BIAS_MEAN + 0.1 * nrm(ks[5], (L, ATTN_HEADS)),
        'w_dw': nrm(ks[6], (L, CONV_KERNEL, CONV_WIDTH)) * CONV_KERNEL ** -0.5,
        'b_dw': 0.02 * nrm(ks[7], (L, CONV_WIDTH)),
        'conv_norm_g': 1.0 + 0.02 * nrm(ks[8], (L, CONV_WIDTH)),
        'conv_norm_b': 0.02 * nrm(ks[9], (L, CONV_WIDTH)),
        'w_out': nrm(ks[10], (L, MIX_WIDTH, D)) * (MIX_WIDTH ** -0.5 * beta),
        'ln1_g': 1.0 + 0.02 * nrm(ks[11], (L, D)),
        'ln1_b': 0.02 * nrm(ks[12], (L, D)),
        'w_router': nrm(ks[13], (L, D, E)) * D ** -0.5,
        'router_bias': 0.01 * nrm(ks[14], (L, E)),
        'w_exp_gate': nrm(ks[15], (L, E, D, F)) * D ** -0.5,
        'w_exp_up': nrm(ks[16], (L, E, D, F)) * D ** -0.5,
        'w_exp_down': nrm(ks[17], (L, E, F, D)) * (F ** -0.5 * beta),
        'w_sh_gate': nrm(ks[18], (L, D, SHARED_FF)) * D ** -0.5,
        'w_sh_up': nrm(ks[19], (L, D, SHARED_FF)) * D ** -0.5,
        'w_sh_down': nrm(ks[20], (L, SHARED_FF, D)) * (SHARED_FF ** -0.5 * beta),
        'ln2_g': 1.0 + 0.02 * nrm(ks[21], (L, D)),
        'ln2_b': 0.02 * nrm(ks[22], (L, D)),
    }


def reference(x, c, w_ada, b_ada, w_in, b_forget, w_dw, b_dw, conv_norm_g, conv_norm_b, w_out,
              ln1_g, ln1_b, w_router, router_bias, w_exp_gate, w_exp_up, w_exp_down,
              w_sh_gate, w_sh_up, w_sh_down, ln2_g, ln2_b):
    B, S, D = x.shape
    alpha = (2.0 * DEPTH) ** 0.25
    splits = [ATTN_WIDTH, 2 * ATTN_WIDTH, 3 * ATTN_WIDTH, 3 * ATTN_WIDTH + ATTN_HEADS,
              3 * ATTN_WIDTH + ATTN_HEADS + CONV_WIDTH]
    for l in range(DEPTH):
        mod = (jax.nn.silu(c) @ w_ada[l] + b_ada[l])[:, None, :]
        shift_a, scale_a, gate_a, shift_f, scale_f, gate_f = jnp.split(mod, 6, axis=-1)

        h = x * (1.0 + scale_a) + shift_a
        proj = h @ w_in[l]
        q, k, v, f_logit, g_val, g_gate = jnp.split(proj, splits, axis=-1)
        log_f = jax.nn.log_sigmoid((f_logit + b_forget[l]).astype(jnp.float32))
        attn = forgetting_attention(q.reshape(B, S, ATTN_HEADS, HEAD_DIM),
                                    k.reshape(B, S, ATTN_HEADS, HEAD_DIM),
                                    v.reshape(B, S, ATTN_HEADS, HEAD_DIM), log_f)
        conv = conformer_conv(g_val, g_gate, w_dw[l], b_dw[l], conv_norm_g[l], conv_norm_b[l])
        mix = jnp.concatenate([attn, conv], axis=-1) @ w_out[l]
        x = layer_norm(alpha * x + gate_a * mix, ln1_g[l], ln1_b[l])

        h2 = x * (1.0 + scale_f) + shift_f
        ffn = moe_ffn(h2, w_router[l], router_bias[l], w_exp_gate[l], w_exp_up[l], w_exp_down[l],
                      w_sh_gate[l], w_sh_up[l], w_sh_down[l])
        x = layer_norm(alpha * x + gate_f * ffn, ln2_g[l], ln2_b[l])
    return x
```

```python
import numpy as np
from contextlib import ExitStack
import concourse.bass as bass
import concourse.mybir as mybir
from concourse.bass_utils import run_bass_kernel_spmd

F32 = mybir.dt.float32
BF16 = mybir.dt.bfloat16
AF = mybir.ActivationFunctionType
ALU = mybir.AluOpType
AX = mybir.AxisListType

D = 4096
KC = 32
S = 8192
SO = 1024
HALO = 32
NH = 16
NE = 64
FF = 512
NCORES = 8
ALPHA = 2.0 ** 0.25
SCALE = 128.0 ** -0.5
EPS = 1e-5
NEGBIG = -30000.0
ND = 48


class Prog:
    def __init__(self, nc, es):
        self.nc = nc
        self.q = {k: [] for k in ("pe", "act", "dve", "pool", "sp")}
        self.sem = {}
        self.semobj = {}
        for e in ("pe", "act", "dve", "pool"):
            h = es.enter_context(nc.semaphore("s_" + e))
            self.sem[e] = h
            self.semobj[e] = h
        self.cnt = {e: 0 for e in self.sem}
        self.waited = {e: {} for e in self.q}
        self.lastw = {}
        self.readers = {}
        self.dpool = []
        for i in range(ND):
            h = es.enter_context(nc.semaphore("d%d" % i))
            self.dpool.append(h)
            self.semobj[("d", i)] = h
        self.dcount = 0
        self.dlast = {}
        self.ninstr = 0

    def _deps(self, reads, writes):
        deps = {}

        def add(k, v):
            if v > deps.get(k, 0):
                deps[k] = v

        for b in reads:
            w = self.lastw.get(b)
            if w:
                add(*w)
        for b in writes:
            w = self.lastw.get(b)
            if w:
                add(*w)
            r = self.readers.get(b)
            if r:
                for k, v in r.items():
                    add(k, v)
        return deps

    def _waits(self, eng, deps, skip=None):
        wd = self.waited[eng]
        for k, v in deps.items():
            if k == skip:
                continue
            if wd.get(k, 0) >= v:
                continue
            wd[k] = v
            h = self.semobj[k]
            self.q[eng].append(lambda e, h=h, v=v: e.wait_ge(h, v))

    def _record(self, tok, reads, writes):
        k, v = tok
        for b in reads:
            r = self.readers.setdefault(b, {})
            if v > r.get(k, 0):
                r[k] = v
        for b in writes:
            self.lastw[b] = tok
            self.readers[b] = {}

    def op(self, eng, fn, reads=(), writes=()):
        deps = self._deps(reads, writes)
        self._waits(eng, deps, skip=("pe" if eng == "pe" else None))
        self.cnt[eng] += 1
        n = self.cnt[eng]
        h = self.sem[eng]
        self.q[eng].append(lambda e, fn=fn, h=h: fn(e).then_inc(h, 1))
        self._record((eng, n), reads, writes)
        self.ninstr += 1

    def dma(self, queue, fn, reads=(), writes=()):
        deps = self._deps(reads, writes)
        i = self.dcount
        self.dcount += 1
        slot = i % ND
        use = i // ND
        k = ("d", slot)
        if use > 0:
            deps[k] = max(deps.get(k, 0), 16 * use)
        self._waits(queue, deps)
        h = self.dpool[slot]
        self.q[queue].append(lambda e, fn=fn, h=h: fn(e).then_inc(h, 16))
        self._record((k, 16 * (use + 1)), reads, writes)
        self.dlast[k] = 16 * (use + 1)
        self.ninstr += 1

    def barrier(self):
        deps = dict(self.dlast)
        for e in self.cnt:
            if self.cnt[e]:
                deps[e] = self.cnt[e]
        for qn in self.q:
            self._waits(qn, dict(deps))

    def finish(self):
        deps = dict(self.dlast)
        for e in self.cnt:
            if self.cnt[e]:
                deps[e] = self.cnt[e]
        self._waits("sp", deps)

    def replay(self):
        nc = self.nc
        q = self.q
        with nc.Block() as block:
            @block.tensor
            def _(e):
                for f in q["pe"]:
                    f(e)

            @block.scalar
            def _(e):
                for f in q["act"]:
                    f(e)

            @block.vector
            def _(e):
                for f in q["dve"]:
                    f(e)

            @block.gpsimd
            def _(e):
                for f in q["pool"]:
                    f(e)

            @block.sync
            def _(e):
                for f in q["sp"]:
                    f(e)


def build(upto=99, debug=()):
    nc = bass.Bass("TRN2", target_bir_lowering=False)
    es = ExitStack()

    declared = []

    def din(name, shape, dt=F32, need=0):
        if upto < need:
            return None
        declared.append(name)
        return nc.dram_tensor(name, list(shape), dt, kind="ExternalInput").ap()

    def dscr(name, shape, dt):
        kind = "ExternalOutput" if name in debug else "Internal"
        return nc.dram_tensor(name, list(shape), dt, kind=kind).ap()

    xT = din("xT", [D, S + HALO])
    xown = din("xown", [SO, D])
    c_l = din("c_l", [128, KC])
    w_ada = din("w_ada", [D, 6 * D])
    b_ada = din("b_ada", [1, 6 * D])
    w_in = din("w_in", [D, 10256])
    bfor = din("bfor", [NH, 1])
    wdw_l = din("wdw_l", [128, 16 * 31])
    bdw_l = din("bdw_l", [128, 16])
    cng_l = din("cng_l", [128, 16])
    cnb_l = din("cnb_l", [128, 16])
    w_out = din("w_out", [D, D], need=4)
    ln1_g = din("ln1_g", [1, D])
    ln1_b = din("ln1_b", [1, D])
    ln2_g = din("ln2_g", [1, D])
    ln2_b = din("ln2_b", [1, D])
    w_router = din("w_router", [D, NE])
    rbias = din("rbias", [1, NE])
    w_eg = din("w_eg", [NE, D, FF], need=6)
    w_eu = din("w_eu", [NE, D, FF], need=6)
    w_ed = din("w_ed", [NE, FF, D], need=7)
    w_sg = din("w_sg", [D, FF], need=6)
    w_su = din("w_su", [D, FF], need=6)
    w_sd = din("w_sd", [FF, D], need=7)
    vis01 = din("vis01", [NH, S])
    visb = din("visb", [NH, S])
    hv = din("hv", [128, 1])
    ident_in = din("ident", [128, 128])
    maskt_in = din("maskt", [128, 8 * 512])
    out = nc.dram_tensor("out", [SO, D], F32, kind="ExternalOutput").ap()

    modd = dscr("modd", [1, 6 * D], F32)
    hT = dscr("hT", [D, S + HALO], BF16)
    QT = dscr("QT", [NH * 128, SO], BF16)
    featT = dscr("featT", [D, SO], BF16)
    y1 = dscr("y1", [SO, D], F32)
    x1 = dscr("x1", [SO, D], F32)
    hidd = dscr("hidd", [65 * 4 * 128, SO], BF16)
    dbgG = dscr("dbgG", [SO, NE], F32)

    P = Prog(nc, es)

    def sb(st, name, shape, dt):
        return st.enter_context(nc.sbuf_tensor(name, list(shape), dt))

    def ps(st, name, shape, dt=F32):
        return st.enter_context(nc.psum_tensor(name, list(shape), dt))

    identf = sb(es, "identf", [128, 128], F32)
    identb = sb(es, "identb", [128, 128], BF16)
    onesb = sb(es, "onesb", [128, 128], BF16)
    onesf = sb(es, "onesf", [128, 128], F32)
    P.dma("sp", lambda e: e.dma_start(out=identf[:], in_=ident_in[:, :]), writes=["identf"])
    P.dma("pool", lambda e: e.dma_start(out=identb[:], in_=ident_in[:, :]), writes=["identb"])
    P.op("dve", lambda e: e.memset(onesb[:], 1.0), writes=["onesb"])
    P.op("dve", lambda e: e.memset(onesf[:], 1.0), writes=["onesf"])

    shsc = sb(es, "shsc", [128, 64], F32)
    nbAll = sb(es, "nbAll", [128, 64 * NH], F32)
    CQ = sb(es, "CQ", [NH, SO], F32)

    P.barrier()
    if upto >= 0:
        with ExitStack() as st:
            csb = sb(st, "csb", [128, KC], F32)
            scb = sb(st, "scb", [128, KC], BF16)
            wt = [sb(st, "wt%d" % i, [128, KC, 512], BF16) for i in range(3)]
            brow = [sb(st, "brow%d" % i, [1, 512], F32) for i in range(2)]
            mrow = [sb(st, "mrow%d" % i, [1, 512], F32) for i in range(2)]
            pm = [ps(st, "pm%d" % i, [1, 512]) for i in range(2)]
            wav = w_ada.rearrange("(kc p) n -> p kc n", p=128)
            P.dma("sp", lambda e: e.dma_start(out=csb[:], in_=c_l[:, :]), writes=["csb"])
            P.op("act", lambda e: e.activation(out=scb[:], in_=csb[:], func=AF.Silu), reads=["csb"], writes=["scb"])
            for ct in range(48):
                sl = ct % 3
                b2 = ct % 2
                P.dma("pool", lambda e, sl=sl, ct=ct: e.dma_start(out=wt[sl][:], in_=wav[:, :, ct * 512:(ct + 1) * 512]),
                      writes=["wt%d" % sl])
                P.dma("sp", lambda e, b2=b2, ct=ct: e.dma_start(out=brow[b2][:], in_=b_ada[0:1, ct * 512:(ct + 1) * 512]),
                      writes=["brow%d" % b2])
                for kc in range(KC):
                    P.op("pe", lambda e, b2=b2, sl=sl, kc=kc: e.matmul(pm[b2][:], lhsT=scb[:, kc:kc + 1], rhs=wt[sl][:, kc, :],
                                                                      start=(kc == 0), stop=(kc == KC - 1)),
                         reads=["wt%d" % sl, "scb"], writes=["pm%d" % b2])
                P.op("dve", lambda e, b2=b2: e.tensor_tensor(out=mrow[b2][:], in0=pm[b2][:], in1=brow[b2][:], op=ALU.add),
                     reads=["pm%d" % b2, "brow%d" % b2], writes=["mrow%d" % b2])
                P.dma("sp", lambda e, b2=b2, ct=ct: e.dma_start(out=modd[0:1, ct * 512:(ct + 1) * 512], in_=mrow[b2][:]),
                      reads=["mrow%d" % b2], writes=["modd"])
            mr = sb(st, "mr", [64, 128], F32)
            pt0 = ps(st, "pt0", [128, 512])
            P.dma("sp", lambda e: e.dma_start(out=mr[:], in_=modd[0, 0:2 * D].rearrange("(r p) -> r p", p=128)),
                  reads=["modd"], writes=["mr"])
            P.op("pe", lambda e: e.transpose(out=pt0[:, 0:64], in_=mr[:], identity=identf[0:64, 0:64]),
                 reads=["mr", "identf"], writes=["pt0"])
            P.op("dve", lambda e: e.tensor_copy(out=shsc[:, 0:32], in_=pt0[:, 0:32]), reads=["pt0"], writes=["shsc"])
            P.op("dve", lambda e: e.tensor_scalar(out=shsc[:, 32:64], in0=pt0[:, 32:64], scalar1=1.0, scalar2=None, op0=ALU.add),
                 reads=["pt0"], writes=["shsc"])

    P.barrier()
    stE = ExitStack()
    EL = sb(stE, "EL", [NH, S], F32)
    if upto >= 1:
        with ExitStack() as st:
            xs = [sb(st, "xs%d" % i, [128, KC, 256], F32) for i in range(2)]
            hb = [sb(st, "hb%d" % i, [128, KC, 256], BF16) for i in range(2)]
            wf = sb(st, "wf", [128, KC, NH], BF16)
            bf_sb = sb(st, "bf_sb", [NH, 1], F32)
            negb = sb(st, "negb", [NH, 1], F32)
            pfl = [ps(st, "pfl%d" % i, [NH, 512]) for i in range(2)]
            xTv = xT.rearrange("(kc p) t -> p kc t", p=128)
            hTv = hT.rearrange("(kc p) t -> p kc t", p=128)
            wiv = w_in.rearrange("(kc p) n -> p kc n", p=128)
            P.dma("pool", lambda e: e.dma_start(out=wf[:], in_=wiv[:, :, 6144:6160]), writes=["wf"])
            P.dma("sp", lambda e: e.dma_start(out=bf_sb[:], in_=bfor[:, :]), writes=["bf_sb"])
            P.op("dve", lambda e: e.tensor_scalar(out=negb[:], in0=bf_sb[:], scalar1=-1.0, scalar2=None, op0=ALU.mult),
                 reads=["bf_sb"], writes=["negb"])
            for t in range(33):
                w = 256 if t < 32 else HALO
                c0 = t * 256
                b2 = t % 2
                P.dma("sp", lambda e, b2=b2, c0=c0, w=w: e.dma_start(out=xs[b2][:, :, 0:w], in_=xTv[:, :, c0:c0 + w]),
                      writes=["xs%d" % b2])
                for kc in range(KC):
                    if kc % 2 == 0:
                        P.op("act", lambda e, b2=b2, kc=kc, w=w: e.activation(
                            out=hb[b2][:, kc, 0:w], in_=xs[b2][:, kc, 0:w], func=AF.Identity,
                            scale=shsc[:, 32 + kc:33 + kc], bias=shsc[:, kc:kc + 1]),
                            reads=["xs%d" % b2, "shsc"], writes=[("hb", b2, kc)])
                    else:
                        P.op("dve", lambda e, b2=b2, kc=kc, w=w: e.tensor_scalar(
                            out=hb[b2][:, kc, 0:w], in0=xs[b2][:, kc, 0:w],
                            scalar1=shsc[:, 32 + kc:33 + kc], scalar2=shsc[:, kc:kc + 1], op0=ALU.mult, op1=ALU.add),
                            reads=["xs%d" % b2, "shsc"], writes=[("hb", b2, kc)])
                P.dma("sp", lambda e, b2=b2, c0=c0, w=w: e.dma_start(out=hTv[:, :, c0:c0 + w], in_=hb[b2][:, :, 0:w]),
                      reads=[("hb", b2, kc) for kc in range(KC)], writes=["hT"])
                if t < 32:
                    for kc in range(KC):
                        P.op("pe", lambda e, b2=b2, kc=kc: e.matmul(pfl[b2][:, 0:256], lhsT=wf[:, kc, :], rhs=hb[b2][:, kc, :],
                                                                  start=(kc == 0), stop=(kc == KC - 1)),
                             reads=["wf", ("hb", b2, kc)], writes=["pfl%d" % b2])
                    P.op("act", lambda e, b2=b2, c0=c0: e.activation(out=EL[:, c0:c0 + 256], in_=pfl[b2][:, 0:256], func=AF.Exp,
                                                                      scale=-1.0, bias=negb[:, 0:1]),
                         reads=["pfl%d" % b2, "negb"], writes=["EL"])
        P.barrier()
        with ExitStack() as st:
            PL = sb(st, "PL", [NH, S], F32)
            tmp = sb(st, "tmpv", [NH, S], F32)
            visx = sb(st, "visx", [NH, S], F32)
            ones16 = sb(st, "ones16", [NH, 2048], F32)
            TL = sb(st, "TL", [NH, 1], F32)
            pnb = ps(st, "pnb", [128, 64 * NH])
            P.op("dve", lambda e: e.memset(ones16[:], 1.0), writes=["ones16"])
            P.op("act", lambda e: e.activation(out=EL[:], in_=EL[:], func=AF.Ln, bias=1.0, scale=1.0), reads=["EL"], writes=["EL"])
            for ch in range(4):
                if ch == 0:
                    P.op("dve", lambda e: e.tensor_tensor_scan(out=PL[:, 0:2048], data0=ones16[:], data1=EL[:, 0:2048],
                                                                initial=0.0, op0=ALU.mult, op1=ALU.add),
                         reads=["EL", "ones16"], writes=["PL"])
                else:
                    P.op("dve", lambda e, ch=ch: e.tensor_tensor_scan(out=PL[:, ch * 2048:(ch + 1) * 2048], data0=ones16[:],
                                                                       data1=EL[:, ch * 2048:(ch + 1) * 2048],
                                                                       initial=PL[:, ch * 2048 - 1:ch * 2048], op0=ALU.mult, op1=ALU.add),
                         reads=["EL", "ones16", "PL"], writes=["PL"])
            P.dma("sp", lambda e: e.dma_start(out=visx[:], in_=vis01[:, :]), writes=["visx"])
            P.op("dve", lambda e: e.tensor_tensor(out=tmp[:], in0=EL[:], in1=visx[:], op=ALU.mult), reads=["EL", "visx"], writes=["tmpv"])
            P.op("dve", lambda e: e.reduce_sum(out=TL[:], in_=tmp[:], axis=AX.X), reads=["tmpv"], writes=["TL"])
            P.op("dve", lambda e: e.tensor_scalar(out=CQ[:], in0=PL[:, 0:SO], scalar1=TL[:, 0:1], scalar2=-1.0, op0=ALU.add, op1=ALU.mult),
                 reads=["PL", "TL"], writes=["CQ"])
            P.dma("sp", lambda e: e.dma_start(out=visx[:], in_=visb[:, :]), reads=[], writes=["visx"])
            P.op("dve", lambda e: e.tensor_tensor(out=tmp[:], in0=PL[:], in1=visx[:], op=ALU.add), reads=["PL", "visx"], writes=["tmpv"])
            P.op("dve", lambda e: e.tensor_scalar(out=tmp[:, 0:SO], in0=tmp[:, 0:SO], scalar1=TL[:, 0:1], scalar2=None, op0=ALU.add),
                 reads=["tmpv", "TL"], writes=["tmpv"])
            for kb in range(64):
                P.op("pe", lambda e, kb=kb: e.transpose(out=pnb[:, kb * NH:(kb + 1) * NH], in_=tmp[:, kb * 128:(kb + 1) * 128],
                                                        identity=identf[0:NH, 0:NH]),
                     reads=["tmpv", "identf"], writes=["pnb"])
            P.op("dve", lambda e: e.tensor_copy(out=nbAll[:], in_=pnb[:]), reads=["pnb"], writes=["nbAll"])

    stE.close()
    P.barrier()
    if upto >= 2:
        with ExitStack() as st:
            hown = sb(st, "hown", [128, KC, HALO + SO], BF16)
            wc = [sb(st, "wc%d" % i, [128, KC, 128], BF16) for i in range(3)]
            qsb = [sb(st, "qsb%d" % i, [128, SO], BF16) for i in range(2)]
            wdw = sb(st, "wdw", [128, 16 * 31], F32)
            bdw = sb(st, "bdw", [128, 16], F32)
            cng = sb(st, "cng", [128, 16], F32)
            cnb = sb(st, "cnb", [128, 16], F32)
            hvs = sb(st, "hvs", [128, 1], F32)
            onesdiv = sb(st, "onesdiv", [128, 128], F32)
            sg = sb(st, "sg", [128, HALO + SO], F32)
            glu = sb(st, "glu", [128, HALO + SO], F32)
            cacc_t = sb(st, "cacc", [128, SO], F32)
            usq = sb(st, "usq", [128, SO], F32)
            msb = sb(st, "msb", [128, 512], F32)
            var = sb(st, "var", [128, 512], F32)
            cen = sb(st, "cen", [128, 512], F32)
            fsb = [sb(st, "fsb%d" % i, [128, SO], BF16) for i in range(2)]
            P0 = ps(st, "P0", [128, 2048])
            P1 = ps(st, "P1", [128, 512])
            P2 = ps(st, "P2", [128, 512])
            P3 = ps(st, "P3", [128, 512])
            hTv = hT.rearrange("(kc p) t -> p kc t", p=128)
            wiv = w_in.rearrange("(kc p) n -> p kc n", p=128)
            P.dma("sp", lambda e: e.dma_start(out=hown[:, :, HALO:HALO + SO], in_=hTv[:, :, 0:SO]), reads=["hT"], writes=["hown"])
            P.dma("sp", lambda e: e.dma_start(out=hown[:, :, 0:HALO], in_=hTv[:, :, S:S + HALO]), reads=["hT"], writes=["hown"])
            P.dma("sp", lambda e: e.dma_start(out=wdw[:], in_=wdw_l[:, :]), writes=["wdw"])
            P.dma("sp", lambda e: e.dma_start(out=bdw[:], in_=bdw_l[:, :]), writes=["bdw"])
            P.dma("sp", lambda e: e.dma_start(out=cng[:], in_=cng_l[:, :]), writes=["cng"])
            P.dma("sp", lambda e: e.dma_start(out=cnb[:], in_=cnb_l[:, :]), writes=["cnb"])
            P.dma("sp", lambda e: e.dma_start(out=hvs[:], in_=hv[:, :]), writes=["hvs"])
            P.op("dve", lambda e: e.memset(onesdiv[:], 1.0 / 128.0), writes=["onesdiv"])
            nload = [0]

            def load_w(col0):
                sl = nload[0] % 3
                nload[0] += 1
                P.dma("pool", lambda e, sl=sl, col0=col0: e.dma_start(out=wc[sl][:], in_=wiv[:, :, col0:col0 + 128]),
                      writes=["wc%d" % sl])
                return sl

            if "dbg_hown" in debug:
                dbg_hown = dscr("dbg_hown", [128, KC * (HALO + SO)], BF16)
                P.dma("sp", lambda e: e.dma_start(out=dbg_hown[:, :], in_=hown[:].rearrange("p k t -> p (k t)")), reads=["hown"], writes=["dbg_hown"])
            for h in range(NH):
                sl = load_w(h * 128)
                if "dbg_wc" in debug and h == 0:
                    dbg_wc = dscr("dbg_wc", [128, KC * 128], BF16)
                    P.dma("sp", lambda e, sl=sl: e.dma_start(out=dbg_wc[:, :], in_=wc[sl][:].rearrange("p k t -> p (k t)")), reads=["wc%d" % sl], writes=["dbg_wc"])
                half = h % 2
                for nt in range(2):
                    for kc in range(KC):
                        P.op("pe", lambda e, sl=sl, kc=kc, nt=nt, half=half: e.matmul(
                            P0[:, half * 1024 + nt * 512: half * 1024 + (nt + 1) * 512], lhsT=wc[sl][:, kc, :],
                            rhs=hown[:, kc, HALO + nt * 512: HALO + (nt + 1) * 512], start=(kc == 0), stop=(kc == KC - 1)),
                            reads=["wc%d" % sl, "hown"], writes=[("P0", half, nt)])
                eng = "act" if h % 2 == 0 else "dve"
                if eng == "act":
                    P.op("act", lambda e, half=half: e.copy(out=qsb[half][:], in_=P0[:, half * 1024:(half + 1) * 1024]),
                         reads=[("P0", half, 0), ("P0", half, 1)], writes=["qsb%d" % half])
                else:
                    P.op("dve", lambda e, half=half: e.tensor_copy(out=qsb[half][:], in_=P0[:, half * 1024:(half + 1) * 1024]),
                         reads=[("P0", half, 0), ("P0", half, 1)], writes=["qsb%d" % half])
                P.dma("sp", lambda e, half=half, h=h: e.dma_start(out=QT[h * 128:(h + 1) * 128, :], in_=qsb[half][:]),
                      reads=["qsb%d" % half], writes=["QT"])
            for c in range(16):
                slv = load_w(6160 + c * 128)
                slg = load_w(8208 + c * 128)
                for half, sl in ((0, slv), (1, slg)):
                    for nt in range(2):
                        for kc in range(KC):
                            P.op("pe", lambda e, sl=sl, kc=kc, nt=nt, half=half: e.matmul(
                                P0[:, half * 1024 + nt * 512: half * 1024 + (nt + 1) * 512], lhsT=wc[sl][:, kc, :],
                                rhs=hown[:, kc, HALO + nt * 512: HALO + (nt + 1) * 512], start=(kc == 0), stop=(kc == KC - 1)),
                                reads=["wc%d" % sl, "hown"], writes=[("P0", half, nt)])
                    for kc in range(KC):
                        P.op("pe", lambda e, sl=sl, kc=kc, half=half: e.matmul(
                            P1[:, half * HALO:(half + 1) * HALO], lhsT=wc[sl][:, kc, :], rhs=hown[:, kc, 0:HALO],
                            start=(kc == 0), stop=(kc == KC - 1)),
                            reads=["wc%d" % sl, "hown"], writes=[("P1", half)])
                P.op("act", lambda e: e.activation(out=sg[:, HALO:HALO + SO], in_=P0[:, 1024:2048], func=AF.Sigmoid),
                     reads=[("P0", 1, 0), ("P0", 1, 1)], writes=["sg"])
                P.op("act", lambda e: e.activation(out=sg[:, 0:HALO], in_=P1[:, HALO:2 * HALO], func=AF.Sigmoid),
                     reads=[("P1", 1)], writes=["sgh"])
                P.op("dve", lambda e: e.tensor_tensor(out=glu[:, HALO:HALO + SO], in0=P0[:, 0:1024], in1=sg[:, HALO:HALO + SO], op=ALU.mult),
                     reads=[("P0", 0, 0), ("P0", 0, 1), "sg"], writes=["glu"])
                P.op("dve", lambda e: e.scalar_tensor_tensor(out=glu[:, 0:HALO], in0=P1[:, 0:HALO], scalar=hvs[:, 0:1], in1=sg[:, 0:HALO],
                                                             op0=ALU.mult, op1=ALU.mult),
                     reads=[("P1", 0), "sgh", "hvs"], writes=["gluh"])
                P.op("dve", lambda e, c=c: e.tensor_scalar(out=cacc_t[:], in0=glu[:, 2:2 + SO], scalar1=wdw[:, c * 31:c * 31 + 1],
                                                          scalar2=bdw[:, c:c + 1], op0=ALU.mult, op1=ALU.add),
                     reads=["glu", "gluh", "wdw", "bdw"], writes=["cacc"])
                for k in range(1, 31):
                    P.op("dve", lambda e, c=c, k=k: e.scalar_tensor_tensor(out=cacc_t[:], in0=glu[:, 2 + k:2 + k + SO],
                                                                          scalar=wdw[:, c * 31 + k:c * 31 + k + 1], in1=cacc_t[:],
                                                                          op0=ALU.mult, op1=ALU.add),
                         reads=["glu", "gluh", "wdw", "cacc"], writes=["cacc"])
                P.op("act", lambda e: e.activation(out=usq[:], in_=cacc_t[:], func=AF.Square), reads=["cacc"], writes=["usq"])
                f2 = c % 2
                for hf in range(2):
                    P.op("pe", lambda e, hf=hf: e.matmul(P2[:], lhsT=onesdiv[:], rhs=cacc_t[:, hf * 512:(hf + 1) * 512], start=True, stop=True),
                         reads=["onesdiv", "cacc"], writes=["P2"])
                    P.op("pe", lambda e, hf=hf: e.matmul(P3[:], lhsT=onesdiv[:], rhs=usq[:, hf * 512:(hf + 1) * 512], start=True, stop=True),
                         reads=["onesdiv", "usq"], writes=["P3"])
                    P.op("act", lambda e: e.copy(out=msb[:], in_=P2[:]), reads=["P2"], writes=["msb"])
                    P.op("dve", lambda e: e.tensor_tensor(out=var[:], in0=msb[:], in1=msb[:], op=ALU.mult), reads=["msb"], writes=["var"])
                    P.op("dve", lambda e: e.tensor_tensor(out=var[:], in0=P3[:], in1=var[:], op=ALU.subtract), reads=["P3", "var"], writes=["var"])
                    P.op("dve", lambda e: e.tensor_scalar(out=var[:], in0=var[:], scalar1=EPS, scalar2=None, op0=ALU.add), reads=["var"], writes=["var"])
                    P.op("act", lambda e: e.activation(out=var[:], in_=var[:], func=AF.Sqrt), reads=["var"], writes=["var"])
                    P.op("dve", lambda e: e.reciprocal(out=var[:], in_=var[:]), reads=["var"], writes=["var"])
                    P.op("dve", lambda e, hf=hf: e.tensor_tensor(out=cen[:], in0=cacc_t[:, hf * 512:(hf + 1) * 512], in1=msb[:], op=ALU.subtract),
                         reads=["cacc", "msb"], writes=["cen"])
                    P.op("dve", lambda e: e.tensor_tensor(out=cen[:], in0=cen[:], in1=var[:], op=ALU.mult), reads=["cen", "var"], writes=["cen"])
                    P.op("dve", lambda e, c=c: e.tensor_scalar(out=cen[:], in0=cen[:], scalar1=cng[:, c:c + 1], scalar2=cnb[:, c:c + 1],
                                                              op0=ALU.mult, op1=ALU.add), reads=["cen", "cng", "cnb"], writes=["cen"])
                    P.op("act", lambda e, hf=hf, f2=f2: e.activation(out=fsb[f2][:, hf * 512:(hf + 1) * 512], in_=cen[:], func=AF.Silu),
                         reads=["cen"], writes=["fsb%d" % f2])
                P.dma("sp", lambda e, c=c, f2=f2: e.dma_start(out=featT[2048 + c * 128: 2048 + (c + 1) * 128, :], in_=fsb[f2][:]),
                      reads=["fsb%d" % f2], writes=["featT"])

    P.barrier()
    if upto >= 3:
        with ExitStack() as st:
            wk = [sb(st, "wk%d" % i, [128, KC, 128], BF16) for i in range(2)]
            wv = [sb(st, "wv%d" % i, [128, KC, 128], BF16) for i in range(2)]
            ht = [sb(st, "ht%d" % i, [128, KC, 512], BF16) for i in range(2)]
            KT = sb(st, "KT", [128, S], BF16)
            V = sb(st, "V", [128, S], BF16)
            QTh = sb(st, "QTh", [128, SO], BF16)
            CQB = sb(st, "CQB", [128, SO], F32)
            sel = sb(st, "sel", [NH, 128], F32)
            X = [sb(st, "X%d" % i, [128, 512], F32) for i in range(2)]
            PT = [sb(st, "PT%d" % i, [128, 512], BF16) for i in range(3)]
            maskt = sb(st, "maskt_sb", [128, 8 * 512], BF16)
            rden = sb(st, "rden", [128, 512], F32)
            osb = [sb(st, "osb%d" % i, [128, 512], BF16) for i in range(2)]
            pk = [ps(st, "pk%d" % i, [128, 512]) for i in range(2)]
            pv = [ps(st, "pv%d" % i, [128, 512]) for i in range(2)]
            pS = [ps(st, "pS%d" % i, [128, 512]) for i in range(2)]
            pO = ps(st, "pO", [128, 512])
            pD = ps(st, "pD", [128, 512])
            hTv = hT.rearrange("(kc p) t -> p kc t", p=128)
            wiv = w_in.rearrange("(kc p) n -> p kc n", p=128)
            P.dma("pool", lambda e: e.dma_start(out=maskt[:], in_=maskt_in[:, :], max_dma_last_dim=2048 * 4), writes=["maskt"])
            ucount = 0
            nheads = NH if upto > 3 or True else 1
            for h in range(nheads):
                w2 = h % 2
                P.dma("pool", lambda e, w2=w2, h=h: e.dma_start(out=wk[w2][:], in_=wiv[:, :, 2048 + h * 128: 2048 + (h + 1) * 128]),
                      writes=["wk%d" % w2])
                P.dma("pool", lambda e, w2=w2, h=h: e.dma_start(out=wv[w2][:], in_=wiv[:, :, 4096 + h * 128: 4096 + (h + 1) * 128]),
                      writes=["wv%d" % w2])
                P.dma("sp", lambda e, h=h: e.dma_start(out=QTh[:], in_=QT[h * 128:(h + 1) * 128, :]), reads=["QT"], writes=["QTh"])
                P.op("dve", lambda e, h=h: e.tensor_scalar(out=sel[:], in0=onesf[0:NH, :], scalar1=identf[0:NH, h:h + 1], scalar2=None, op0=ALU.mult),
                     reads=["onesf", "identf"], writes=["sel"])
                for hf in range(2):
                    P.op("pe", lambda e, hf=hf: e.matmul(pS[hf][:], lhsT=sel[:], rhs=CQ[:, hf * 512:(hf + 1) * 512], start=True, stop=True),
                         reads=["sel", "CQ"], writes=["pS%d" % hf])
                    P.op("act", lambda e, hf=hf: e.copy(out=CQB[:, hf * 512:(hf + 1) * 512], in_=pS[hf][:]),
                         reads=["pS%d" % hf], writes=[("CQB", hf)])
                for tt in range(16):
                    b2 = tt % 2
                    P.dma("sp", lambda e, b2=b2, tt=tt: e.dma_start(out=ht[b2][:], in_=hTv[:, :, tt * 512:(tt + 1) * 512]),
                          reads=["hT"], writes=["ht%d" % b2])
                    for kc in range(KC):
                        P.op("pe", lambda e, b2=b2, kc=kc, w2=w2: e.matmul(pk[b2][:], lhsT=wk[w2][:, kc, :], rhs=ht[b2][:, kc, :],
                                                                          start=(kc == 0), stop=(kc == KC - 1)),
                             reads=["wk%d" % w2, "ht%d" % b2], writes=["pk%d" % b2])
                    P.op("act", lambda e, b2=b2, tt=tt: e.copy(out=KT[:, tt * 512:(tt + 1) * 512], in_=pk[b2][:]),
                         reads=["pk%d" % b2], writes=[("KT", tt)])
                    for j in range(4):
                        for kc in range(KC):
                            P.op("pe", lambda e, b2=b2, kc=kc, w2=w2, j=j: e.matmul(pv[b2][:, j * 128:(j + 1) * 128],
                                                                                   lhsT=ht[b2][:, kc, j * 128:(j + 1) * 128], rhs=wv[w2][:, kc, :],
                                                                                   start=(kc == 0), stop=(kc == KC - 1)),
                                 reads=["wv%d" % w2, "ht%d" % b2], writes=["pv%d" % b2])
                    P.op("dve", lambda e, b2=b2, tt=tt: e.tensor_copy(out=V[:, tt * 512:(tt + 1) * 512], in_=pv[b2][:]),
                         reads=["pv%d" % b2], writes=[("V", tt)])
                for half in range(2):
                    kbs = [kb for kb in range(8) if kb // 4 <= half] + list(range(8, 64))
                    units = []
                    for idx, kb in enumerate(kbs):
                        units.append((ucount, idx, kb))
                        ucount += 1

                    def emit_S(u, idx, kb, half=half):
                        P.op("pe", lambda e, u=u, kb=kb, half=half: e.matmul(pS[u % 2][:], lhsT=KT[:, kb * 128:(kb + 1) * 128],
                                                                            rhs=QTh[:, half * 512:(half + 1) * 512], start=True, stop=True),
                             reads=[("KT", kb // 4), "QTh"], writes=["pS%d" % (u % 2)])

                    def emit_rest(u, idx, kb, half=half, h=h, n=len(kbs)):
                        P.op("dve", lambda e, u=u, half=half: e.scalar_tensor_tensor(out=X[u % 2][:], in0=pS[u % 2][:], scalar=SCALE,
                                                                                   in1=CQB[:, half * 512:(half + 1) * 512], op0=ALU.mult, op1=ALU.add),
                             reads=["pS%d" % (u % 2), ("CQB", half)], writes=["X%d" % (u % 2)])
                        if kb < 8 and kb // 4 == half:
                            P.op("pool", lambda e, u=u, kb=kb: e.tensor_tensor(out=X[u % 2][:], in0=X[u % 2][:], in1=maskt[:, kb * 512:(kb + 1) * 512], op=ALU.add),
                                 reads=["X%d" % (u % 2), "maskt"], writes=["X%d" % (u % 2)])
                        P.op("act", lambda e, u=u, kb=kb, h=h: e.activation(out=PT[u % 3][:], in_=X[u % 2][:], func=AF.Exp,
                                                                          bias=nbAll[:, kb * NH + h: kb * NH + h + 1], scale=1.0),
                             reads=["X%d" % (u % 2), "nbAll"], writes=["PT%d" % (u % 3)])
                        P.op("pe", lambda e, u=u, kb=kb, idx=idx, n=n: e.matmul(pO[:], lhsT=V[:, kb * 128:(kb + 1) * 128], rhs=PT[u % 3][:],
                                                                               start=(idx == 0), stop=(idx == n - 1)),
                             reads=[("V", kb // 4), "PT%d" % (u % 3)], writes=["pO"])
                        P.op("pe", lambda e, u=u, idx=idx, n=n: e.matmul(pD[:], lhsT=onesb[:], rhs=PT[u % 3][:],
                                                                        start=(idx == 0), stop=(idx == n - 1)),
                             reads=["onesb", "PT%d" % (u % 3)], writes=["pD"])

                    emit_S(*units[0])
                    for i, un in enumerate(units):
                        if i + 1 < len(units):
                            emit_S(*units[i + 1])
                        emit_rest(*un)
                    o2 = (h * 2 + half) % 2
                    P.op("dve", lambda e: e.reciprocal(out=rden[:], in_=pD[:]), reads=["pD"], writes=["rden"])
                    P.op("dve", lambda e, o2=o2: e.tensor_tensor(out=osb[o2][:], in0=pO[:], in1=rden[:], op=ALU.mult),
                         reads=["pO", "rden"], writes=["osb%d" % o2])
                    P.dma("sp", lambda e, o2=o2, h=h, half=half: e.dma_start(out=featT[h * 128:(h + 1) * 128, half * 512:(half + 1) * 512], in_=osb[o2][:]),
                          reads=["osb%d" % o2], writes=["featT"])

    P.barrier()
    if upto >= 4:
        with ExitStack() as st:
            Fm = sb(st, "Fm", [128, KC, SO], BF16)
            gab = sb(st, "gab", [128, D], F32)
            wo = [sb(st, "wo%d" % i, [128, KC, 512], BF16) for i in range(2)]
            xt = [sb(st, "xt%d" % i, [128, 512], F32) for i in range(3)]
            ysb = [sb(st, "ysb%d" % i, [128, 512], F32) for i in range(3)]
            py = [ps(st, "py%d" % i, [128, 512]) for i in range(4)]
            fTv = featT.rearrange("(kc p) t -> p kc t", p=128)
            wov = w_out.rearrange("(kc p) n -> p kc n", p=128)
            for q4 in range(4):
                P.dma("sp", lambda e, q4=q4: e.dma_start(out=Fm[:, q4 * 8:(q4 + 1) * 8, :], in_=fTv[:, q4 * 8:(q4 + 1) * 8, :]),
                      reads=["featT"], writes=["Fm"])
            P.dma("sp", lambda e: e.dma_start(out=gab[:], in_=modd[0, 2 * D:3 * D].partition_broadcast(128)), reads=["modd"], writes=["gab"])
            it = 0
            for dt in range(8):
                w2 = dt % 2
                P.dma("pool", lambda e, w2=w2, dt=dt: e.dma_start(out=wo[w2][:], in_=wov[:, :, dt * 512:(dt + 1) * 512]), writes=["wo%d" % w2])
                for tt in range(8):
                    x3 = it % 3
                    p4 = it % 4
                    it += 1
                    P.dma("sp", lambda e, x3=x3, tt=tt, dt=dt: e.dma_start(out=xt[x3][:], in_=xown[tt * 128:(tt + 1) * 128, dt * 512:(dt + 1) * 512]),
                          writes=["xt%d" % x3])
                    for kc in range(KC):
                        P.op("pe", lambda e, p4=p4, kc=kc, tt=tt, w2=w2: e.matmul(py[p4][:], lhsT=Fm[:, kc, tt * 128:(tt + 1) * 128], rhs=wo[w2][:, kc, :],
                                                                                 start=(kc == 0), stop=(kc == KC - 1)),
                             reads=["Fm", "wo%d" % w2], writes=["py%d" % p4])
                    P.op("dve", lambda e, p4=p4, x3=x3, dt=dt: e.tensor_tensor(out=ysb[x3][:], in0=py[p4][:], in1=gab[:, dt * 512:(dt + 1) * 512], op=ALU.mult),
                         reads=["py%d" % p4, "gab"], writes=["ysb%d" % x3])
                    P.op("dve", lambda e, x3=x3: e.scalar_tensor_tensor(out=ysb[x3][:], in0=xt[x3][:], scalar=ALPHA, in1=ysb[x3][:], op0=ALU.mult, op1=ALU.add),
                         reads=["xt%d" % x3, "ysb%d" % x3], writes=["ysb%d" % x3])
                    P.dma("sp", lambda e, x3=x3, tt=tt, dt=dt: e.dma_start(out=y1[tt * 128:(tt + 1) * 128, dt * 512:(dt + 1) * 512], in_=ysb[x3][:]),
                          reads=["ysb%d" % x3], writes=["y1"])

    P.barrier()
    stM = ExitStack()
    h2T = sb(stM, "h2T", [128, KC, SO], BF16)
    GT = sb(stM, "GT", [NE, SO], F32)

    def layer_norm_tile(st_tiles, yt, key, gkey, bkey, gb, bb):
        stats, mv, rstd = st_tiles
        for c8 in range(8):
            P.op("dve", lambda e, c8=c8: e.bn_stats(out=stats[:, c8 * 6:(c8 + 1) * 6], in_=yt[:, c8 * 512:(c8 + 1) * 512]),
                 reads=[key], writes=["stats"])
        P.op("dve", lambda e: e.bn_aggr(out=mv[:], in_=stats[:]), reads=["stats"], writes=["mv"])
        P.op("dve", lambda e: e.tensor_scalar(out=rstd[:], in0=mv[:, 1:2], scalar1=EPS, scalar2=None, op0=ALU.add), reads=["mv"], writes=["rstd"])
        P.op("act", lambda e: e.activation(out=rstd[:], in_=rstd[:], func=AF.Sqrt), reads=["rstd"], writes=["rstd"])
        P.op("dve", lambda e: e.reciprocal(out=rstd[:], in_=rstd[:]), reads=["rstd"], writes=["rstd"])
        P.op("dve", lambda e: e.tensor_scalar(out=yt[:], in0=yt[:], scalar1=mv[:, 0:1], scalar2=rstd[:, 0:1], op0=ALU.subtract, op1=ALU.mult),
             reads=[key, "mv", "rstd"], writes=[key])
        P.op("dve", lambda e: e.tensor_tensor(out=yt[:], in0=yt[:], in1=gb[:], op=ALU.mult), reads=[key, gkey], writes=[key])
        P.op("pool", lambda e: e.tensor_tensor(out=yt[:], in0=yt[:], in1=bb[:], op=ALU.add), reads=[key, bkey], writes=[key])

    if upto >= 5:
        with ExitStack() as st:
            g1b = sb(st, "g1b", [128, D], F32)
            b1b = sb(st, "b1b", [128, D], F32)
            scf = sb(st, "scf", [128, D], F32)
            shf = sb(st, "shf", [128, D], F32)
            yt = [sb(st, "yt%d" % i, [128, D], F32) for i in range(2)]
            h2t = [sb(st, "h2t%d" % i, [128, D], F32) for i in range(2)]
            stats = sb(st, "stats", [128, 48], F32)
            mv = sb(st, "mv", [128, 2], F32)
            rstd = sb(st, "rstd", [128, 1], F32)
            ptr = [ps(st, "ptr%d" % i, [128, 512]) for i in range(4)]
            P.dma("sp", lambda e: e.dma_start(out=g1b[:], in_=ln1_g[0, :].partition_broadcast(128)), writes=["g1b"])
            P.dma("sp", lambda e: e.dma_start(out=b1b[:], in_=ln1_b[0, :].partition_broadcast(128)), writes=["b1b"])
            P.dma("sp", lambda e: e.dma_start(out=scf[:], in_=modd[0, 4 * D:5 * D].partition_broadcast(128)), reads=["modd"], writes=["scf"])
            P.dma("sp", lambda e: e.dma_start(out=shf[:], in_=modd[0, 3 * D:4 * D].partition_broadcast(128)), reads=["modd"], writes=["shf"])
            P.op("pool", lambda e: e.tensor_scalar(out=scf[:], in0=scf[:], scalar1=1.0, scalar2=None, op0=ALU.add), reads=["scf"], writes=["scf"])
            npt = 0
            for tt in range(8):
                b2 = tt % 2
                P.dma("sp", lambda e, b2=b2, tt=tt: e.dma_start(out=yt[b2][:], in_=y1[tt * 128:(tt + 1) * 128, :]), reads=["y1"], writes=["yt%d" % b2])
                layer_norm_tile((stats, mv, rstd), yt[b2], "yt%d" % b2, "g1b", "b1b", g1b, b1b)
                P.dma("sp", lambda e, b2=b2, tt=tt: e.dma_start(out=x1[tt * 128:(tt + 1) * 128, :], in_=yt[b2][:]), reads=["yt%d" % b2], writes=["x1"])
                P.op("dve", lambda e, b2=b2: e.tensor_tensor(out=h2t[b2][:], in0=yt[b2][:], in1=scf[:], op=ALU.mult),
                     reads=["yt%d" % b2, "scf"], writes=["h2t%d" % b2])
                P.op("pool", lambda e, b2=b2: e.tensor_tensor(out=h2t[b2][:], in0=h2t[b2][:], in1=shf[:], op=ALU.add),
                     reads=["h2t%d" % b2, "shf"], writes=["h2t%d" % b2])
                for g4 in range(8):
                    p4 = npt % 4
                    npt += 1
                    for j in range(4):
                        kc = g4 * 4 + j
                        P.op("pe", lambda e, p4=p4, j=j, kc=kc, b2=b2: e.transpose(out=ptr[p4][:, j * 128:(j + 1) * 128], in_=h2t[b2][:, kc * 128:(kc + 1) * 128],
                                                                                identity=identf[:]),
                             reads=["h2t%d" % b2, "identf"], writes=["ptr%d" % p4])
                    dst = h2T[:, g4 * 4:(g4 + 1) * 4, tt * 128:(tt + 1) * 128]
                    src = ptr[p4][:].rearrange("p (j t) -> p j t", j=4)
                    if g4 % 2 == 0:
                        P.op("act", lambda e, dst=dst, src=src: e.copy(out=dst, in_=src), reads=["ptr%d" % p4], writes=[("h2T", tt)])
                    else:
                        P.op("dve", lambda e, dst=dst, src=src: e.tensor_copy(out=dst, in_=src), reads=["ptr%d" % p4], writes=[("h2T", tt)])
        P.barrier()
        with ExitStack() as st:
            wr = sb(st, "wr", [128, KC, NE], BF16)
            rbb = sb(st, "rbb", [128, NE], F32)
            sc = sb(st, "rsc", [128, NE], F32)
            bi = sb(st, "rbi", [128, NE], F32)
            m88 = sb(st, "m88", [128, 64], F32)
            gs = sb(st, "gs", [128, 8], F32)
            m8 = sb(st, "m8", [128, 8], F32)
            gm = sb(st, "gm", [128, 8], F32)
            gn = sb(st, "gn", [128, 8], F32)
            msk = sb(st, "msk", [128, NE], F32)
            selr = sb(st, "selr", [128, NE], F32)
            ssum = sb(st, "ssum", [128, 1], F32)
            G = sb(st, "G", [128, 8 * NE], F32)
            pr = [ps(st, "pr%d" % i, [128, 512]) for i in range(2)]
            pgt = ps(st, "pgt", [NE, SO])
            P.dma("pool", lambda e: e.dma_start(out=wr[:], in_=w_router.rearrange("(kc p) n -> p kc n", p=128)), writes=["wr"])
            P.dma("sp", lambda e: e.dma_start(out=rbb[:], in_=rbias[0, :].partition_broadcast(128)), writes=["rbb"])
            for tt in range(8):
                b2 = tt % 2
                for kc in range(KC):
                    P.op("pe", lambda e, b2=b2, kc=kc, tt=tt: e.matmul(pr[b2][:, 0:NE], lhsT=h2T[:, kc, tt * 128:(tt + 1) * 128], rhs=wr[:, kc, :],
                                                                      start=(kc == 0), stop=(kc == KC - 1)),
                         reads=[("h2T", tt), "wr"], writes=["pr%d" % b2])
                P.op("act", lambda e, b2=b2: e.activation(out=sc[:], in_=pr[b2][:, 0:NE], func=AF.Sigmoid), reads=["pr%d" % b2], writes=["rsc"])
                P.op("dve", lambda e: e.tensor_tensor(out=bi[:], in0=sc[:], in1=rbb[:], op=ALU.add), reads=["rsc", "rbb"], writes=["rbi"])
                for g in range(8):
                    P.op("dve", lambda e, g=g: e.max(out=m88[:, g * 8:(g + 1) * 8], in_=bi[:, g * 8:(g + 1) * 8]), reads=["rbi"], writes=["m88"])
                m3 = m88[:].rearrange("p (g k) -> p g k", k=8)
                P.op("dve", lambda e, m3=m3: e.tensor_tensor(out=gs[:], in0=m3[:, :, 0], in1=m3[:, :, 1], op=ALU.add), reads=["m88"], writes=["gs"])
                P.op("dve", lambda e: e.max(out=m8[:], in_=gs[:]), reads=["gs"], writes=["m8"])
                P.op("dve", lambda e: e.tensor_scalar(out=gm[:], in0=gs[:], scalar1=m8[:, 3:4], scalar2=None, op0=ALU.is_ge), reads=["gs", "m8"], writes=["gm"])
                P.op("dve", lambda e: e.tensor_scalar(out=gn[:], in0=gm[:], scalar1=-1.0, scalar2=-NEGBIG, op0=ALU.add, op1=ALU.mult), reads=["gm"], writes=["gn"])
                for g in range(8):
                    P.op("dve", lambda e, g=g: e.tensor_scalar(out=msk[:, g * 8:(g + 1) * 8], in0=bi[:, g * 8:(g + 1) * 8], scalar1=gm[:, g:g + 1],
                                                              scalar2=gn[:, g:g + 1], op0=ALU.mult, op1=ALU.add), reads=["rbi", "gm", "gn"], writes=["msk"])
                P.op("dve", lambda e: e.max(out=m8[:], in_=msk[:]), reads=["msk"], writes=["m8"])
                P.op("dve", lambda e: e.tensor_scalar(out=selr[:], in0=msk[:], scalar1=m8[:, 5:6], scalar2=None, op0=ALU.is_ge), reads=["msk", "m8"], writes=["selr"])
                P.op("dve", lambda e: e.tensor_tensor(out=selr[:], in0=selr[:], in1=sc[:], op=ALU.mult), reads=["selr", "rsc"], writes=["selr"])
                P.op("dve", lambda e: e.reduce_sum(out=ssum[:], in_=selr[:], axis=AX.X), reads=["selr"], writes=["ssum"])
                P.op("dve", lambda e: e.reciprocal(out=ssum[:], in_=ssum[:]), reads=["ssum"], writes=["ssum"])
                P.op("dve", lambda e, tt=tt: e.tensor_scalar(out=G[:, tt * NE:(tt + 1) * NE], in0=selr[:], scalar1=ssum[:, 0:1], scalar2=2.5, op0=ALU.mult, op1=ALU.mult),
                     reads=["selr", "ssum"], writes=[("G", tt)])
                if "dbgG" in debug:
                    P.dma("sp", lambda e, tt=tt: e.dma_start(out=dbgG[tt * 128:(tt + 1) * 128, :], in_=G[:, tt * NE:(tt + 1) * NE]), reads=[("G", tt)], writes=["dbgG"])
                P.op("pe", lambda e, tt=tt: e.transpose(out=pgt[:, tt * 128:(tt + 1) * 128], in_=G[:, tt * NE:(tt + 1) * NE], identity=identf[:]),
                     reads=[("G", tt), "identf"], writes=["pgt"])
            P.op("dve", lambda e: e.tensor_copy(out=GT[:], in_=pgt[:]), reads=["pgt"], writes=["GT"])

    P.barrier()
    if upto >= 6:
        with ExitStack() as st:
            wg = [sb(st, "wg%d" % i, [128, KC, 128], BF16) for i in range(2)]
            wu = [sb(st, "wu%d" % i, [128, KC, 128], BF16) for i in range(2)]
            GB = [sb(st, "GB%d" % i, [128, SO], F32) for i in range(2)]
            sele = sb(st, "sele", [NE, 128], F32)
            sgt = [sb(st, "sgt%d" % i, [128, 512], F32) for i in range(2)]
            hh = [sb(st, "hh%d" % i, [128, 512], F32) for i in range(2)]
            hidG = [sb(st, "hidG%d" % i, [128, SO], BF16) for i in range(2)]
            pg = [ps(st, "pg%d" % i, [128, 512]) for i in range(3)]
            pu = [ps(st, "pu%d" % i, [128, 512]) for i in range(3)]
            pG = [ps(st, "pG%d" % i, [128, 512]) for i in range(2)]
            nhalf = 0
            nexp = 65 if upto > 6 or True else 2
            for ex in range(nexp):
                g2 = ex % 2
                if ex < NE:
                    P.op("dve", lambda e, ex=ex: e.tensor_scalar(out=sele[:], in0=onesf[0:NE, :], scalar1=identf[0:NE, ex:ex + 1], scalar2=None, op0=ALU.mult),
                         reads=["onesf", "identf"], writes=["sele"])
                    for hf in range(2):
                        P.op("pe", lambda e, hf=hf: e.matmul(pG[hf][:], lhsT=sele[:], rhs=GT[:, hf * 512:(hf + 1) * 512], start=True, stop=True),
                             reads=["sele", "GT"], writes=["pG%d" % hf])
                        P.op("act", lambda e, hf=hf, g2=g2: e.copy(out=GB[g2][:, hf * 512:(hf + 1) * 512], in_=pG[hf][:]),
                             reads=["pG%d" % hf], writes=[("GB", g2, hf)])
                    wgv = w_eg[ex].rearrange("(kc p) f -> p kc f", p=128)
                    wuv = w_eu[ex].rearrange("(kc p) f -> p kc f", p=128)
                else:
                    wgv = w_sg.rearrange("(kc p) f -> p kc f", p=128)
                    wuv = w_su.rearrange("(kc p) f -> p kc f", p=128)
                for fc in range(4):
                    u = ex * 4 + fc
                    w2 = u % 2
                    P.dma("pool", lambda e, w2=w2, wgv=wgv, fc=fc: e.dma_start(out=wg[w2][:], in_=wgv[:, :, fc * 128:(fc + 1) * 128]), writes=["wg%d" % w2])
                    P.dma("pool", lambda e, w2=w2, wuv=wuv, fc=fc: e.dma_start(out=wu[w2][:], in_=wuv[:, :, fc * 128:(fc + 1) * 128]), writes=["wu%d" % w2])
                    for hf in range(2):
                        p3 = nhalf % 3
                        s2 = nhalf % 2
                        nhalf += 1
                        for kc in range(KC):
                            P.op("pe", lambda e, p3=p3, w2=w2, kc=kc, hf=hf: e.matmul(pg[p3][:], lhsT=wg[w2][:, kc, :], rhs=h2T[:, kc, hf * 512:(hf + 1) * 512],
                                                                                     start=(kc == 0), stop=(kc == KC - 1)),
                                 reads=["wg%d" % w2] + [("h2T", t) for t in range(hf * 4, hf * 4 + 4)], writes=["pg%d" % p3])
                        for kc in range(KC):
                            P.op("pe", lambda e, p3=p3, w2=w2, kc=kc, hf=hf: e.matmul(pu[p3][:], lhsT=wu[w2][:, kc, :], rhs=h2T[:, kc, hf * 512:(hf + 1) * 512],
                                                                                     start=(kc == 0), stop=(kc == KC - 1)),
                                 reads=["wu%d" % w2] + [("h2T", t) for t in range(hf * 4, hf * 4 + 4)], writes=["pu%d" % p3])
                        P.op("act", lambda e, p3=p3, s2=s2: e.activation(out=sgt[s2][:], in_=pg[p3][:], func=AF.Silu), reads=["pg%d" % p3], writes=["sgt%d" % s2])
                        if ex < NE:
                            P.op("dve", lambda e, p3=p3, s2=s2: e.tensor_tensor(out=hh[s2][:], in0=pu[p3][:], in1=sgt[s2][:], op=ALU.mult),
                                 reads=["pu%d" % p3, "sgt%d" % s2], writes=["hh%d" % s2])
                            P.op("pool", lambda e, s2=s2, w2=w2, hf=hf, g2=g2: e.tensor_tensor(out=hidG[w2][:, hf * 512:(hf + 1) * 512], in0=hh[s2][:],
                                                                                              in1=GB[g2][:, hf * 512:(hf + 1) * 512], op=ALU.mult),
                                 reads=["hh%d" % s2, ("GB", g2, hf)], writes=[("hidG", w2, hf)])
                        else:
                            P.op("dve", lambda e, p3=p3, s2=s2, w2=w2, hf=hf: e.tensor_tensor(out=hidG[w2][:, hf * 512:(hf + 1) * 512], in0=pu[p3][:], in1=sgt[s2][:], op=ALU.mult),
                                 reads=["pu%d" % p3, "sgt%d" % s2], writes=[("hidG", w2, hf)])
                    P.dma("sp", lambda e, w2=w2, u=u: e.dma_start(out=hidd[u * 128:(u + 1) * 128, :], in_=hidG[w2][:]),
                          reads=[("hidG", w2, 0), ("hidG", w2, 1)], writes=[("hidd", u)])

    stM.close()
    P.barrier()
    if upto >= 7:
        with ExitStack() as st:
            acc = sb(st, "acc", [128, 8 * D], F32)
            with ExitStack() as st2:
                wd = [sb(st2, "wd%d" % i, [128, 4, 2048], BF16) for i in range(3)]
                hin = [sb(st2, "hin%d" % i, [128, 4, SO], BF16) for i in range(2)]
                tmpa = [sb(st2, "tmpa%d" % i, [128, 512], F32) for i in range(2)]
                pd = [ps(st2, "pd%d" % i, [128, 512]) for i in range(4)]
                nwd = 0
                ngrp = 0
                for ex in range(65):
                    h2 = ex % 2
                    P.dma("sp", lambda e, h2=h2, ex=ex: e.dma_start(out=hin[h2][:], in_=hidd[ex * 512:(ex + 1) * 512, :].rearrange("(f p) t -> p f t", p=128)),
                          reads=[("hidd", ex * 4 + f) for f in range(4)], writes=["hin%d" % h2])
                    wdv = (w_ed[ex] if ex < NE else w_sd).rearrange("(f p) d -> p f d", p=128)
                    for dh in range(2):
                        w3 = nwd % 3
                        nwd += 1
                        P.dma("pool", lambda e, w3=w3, wdv=wdv, dh=dh: e.dma_start(out=wd[w3][:], in_=wdv[:, :, dh * 2048:(dh + 1) * 2048]), writes=["wd%d" % w3])
                        for tt in range(8):
                            for dq in range(4):
                                p4 = ngrp % 4
                                for f in range(4):
                                    P.op("pe", lambda e, p4=p4, h2=h2, f=f, tt=tt, w3=w3, dq=dq: e.matmul(pd[p4][:], lhsT=hin[h2][:, f, tt * 128:(tt + 1) * 128],
                                                                                                       rhs=wd[w3][:, f, dq * 512:(dq + 1) * 512], start=(f == 0), stop=(f == 3)),
                                         reads=["hin%d" % h2, "wd%d" % w3], writes=["pd%d" % p4])
                                col = tt * D + dh * 2048 + dq * 512
                                akey = ("acc", tt, dh, dq)
                                if ex == 0:
                                    P.op("act", lambda e, p4=p4, col=col: e.copy(out=acc[:, col:col + 512], in_=pd[p4][:]), reads=["pd%d" % p4], writes=[akey])
                                elif ngrp % 3 != 2:
                                    P.op("dve", lambda e, p4=p4, col=col: e.tensor_tensor(out=acc[:, col:col + 512], in0=pd[p4][:], in1=acc[:, col:col + 512], op=ALU.add),
                                         reads=["pd%d" % p4, akey], writes=[akey])
                                else:
                                    t2 = (ngrp // 3) % 2
                                    P.op("act", lambda e, p4=p4, t2=t2: e.copy(out=tmpa[t2][:], in_=pd[p4][:]), reads=["pd%d" % p4], writes=["tmpa%d" % t2])
                                    P.op("pool", lambda e, t2=t2, col=col: e.tensor_tensor(out=acc[:, col:col + 512], in0=tmpa[t2][:], in1=acc[:, col:col + 512], op=ALU.add),
                                         reads=["tmpa%d" % t2, akey], writes=[akey])
                                ngrp += 1
            P.barrier()
            with ExitStack() as st2:
                gfb = sb(st2, "gfb", [128, D], F32)
                g2b = sb(st2, "g2b", [128, D], F32)
                b2b = sb(st2, "b2b", [128, D], F32)
                x1t = [sb(st2, "x1t%d" % i, [128, D], F32) for i in range(1)]
                stats = sb(st2, "stats2", [128, 48], F32)
                mv = sb(st2, "mv2", [128, 2], F32)
                rstd = sb(st2, "rstd2", [128, 1], F32)
                P.dma("sp", lambda e: e.dma_start(out=gfb[:], in_=modd[0, 5 * D:6 * D].partition_broadcast(128)), reads=["modd"], writes=["gfb"])
                P.dma("sp", lambda e: e.dma_start(out=g2b[:], in_=ln2_g[0, :].partition_broadcast(128)), writes=["g2b"])
                P.dma("sp", lambda e: e.dma_start(out=b2b[:], in_=ln2_b[0, :].partition_broadcast(128)), writes=["b2b"])
                for tt in range(8):
                    b2 = 0
                    at = acc[:, tt * D:(tt + 1) * D]
                    akeys = [("acc", tt, dh, dq) for dh in range(2) for dq in range(4)]
                    key = ("accrow", tt)
                    P.dma("sp", lambda e, b2=b2, tt=tt: e.dma_start(out=x1t[b2][:], in_=x1[tt * 128:(tt + 1) * 128, :]), reads=["x1"], writes=["x1t%d" % b2])
                    P.op("dve", lambda e, at=at: e.tensor_tensor(out=at, in0=at, in1=gfb[:], op=ALU.mult), reads=akeys + ["gfb"], writes=akeys + [key])
                    P.op("dve", lambda e, at=at, b2=b2: e.scalar_tensor_tensor(out=at, in0=x1t[b2][:], scalar=ALPHA, in1=at, op0=ALU.mult, op1=ALU.add),
                         reads=[key, "x1t%d" % b2], writes=[key])

                    class _T:
                        def __init__(self, ap):
                            self.ap = ap

                        def __getitem__(self, idx):
                            if idx == slice(None):
                                return self.ap
                            return self.ap[idx]
                    layer_norm_tile((stats, mv, rstd), _T(at), key, "g2b", "b2b", g2b, b2b)
                    P.dma("sp", lambda e, at=at, tt=tt: e.dma_start(out=out[tt * 128:(tt + 1) * 128, :], in_=at), reads=[key], writes=["out"])
    else:
        with ExitStack() as st:
            z = sb(st, "zt", [128, D], F32)
            P.op("dve", lambda e: e.memset(z[:], 0.0), writes=["zt"])
            for tt in range(8):
                P.dma("sp", lambda e, tt=tt: e.dma_start(out=out[tt * 128:(tt + 1) * 128, :], in_=z[:]), reads=["zt"], writes=["out"])

    P.finish()
    P.replay()
    es.close()
    P.declared = declared
    return nc, P


def make_in_maps(inputs):
    f32 = np.float32
    x = np.asarray(inputs["x"], f32)[0]
    xT_full = np.ascontiguousarray(x.T)
    sh = {
        "c_l": np.ascontiguousarray(np.asarray(inputs["c"], f32).reshape(KC, 128).T),
        "w_ada": np.asarray(inputs["w_ada"], f32)[0],
        "b_ada": np.asarray(inputs["b_ada"], f32).reshape(1, 6 * D),
        "w_in": np.asarray(inputs["w_in"], f32)[0],
        "bfor": np.asarray(inputs["b_forget"], f32).reshape(NH, 1),
        "wdw_l": np.ascontiguousarray(np.asarray(inputs["w_dw"], f32)[0].reshape(31, 16, 128).transpose(2, 1, 0).reshape(128, 16 * 31)),
        "bdw_l": np.ascontiguousarray(np.asarray(inputs["b_dw"], f32).reshape(16, 128).T),
        "cng_l": np.ascontiguousarray(np.asarray(inputs["conv_norm_g"], f32).reshape(16, 128).T),
        "cnb_l": np.ascontiguousarray(np.asarray(inputs["conv_norm_b"], f32).reshape(16, 128).T),
        "w_out": np.asarray(inputs["w_out"], f32)[0],
        "ln1_g": np.asarray(inputs["ln1_g"], f32).reshape(1, D),
        "ln1_b": np.asarray(inputs["ln1_b"], f32).reshape(1, D),
        "ln2_g": np.asarray(inputs["ln2_g"], f32).reshape(1, D),
        "ln2_b": np.asarray(inputs["ln2_b"], f32).reshape(1, D),
        "w_router": np.asarray(inputs["w_router"], f32)[0],
        "rbias": np.asarray(inputs["router_bias"], f32).reshape(1, NE),
        "w_eg": np.asarray(inputs["w_exp_gate"], f32)[0],
        "w_eu": np.asarray(inputs["w_exp_up"], f32)[0],
        "w_ed": np.asarray(inputs["w_exp_down"], f32)[0],
        "w_sg": np.asarray(inputs["w_sh_gate"], f32)[0],
        "w_su": np.asarray(inputs["w_sh_up"], f32)[0],
        "w_sd": np.asarray(inputs["w_sh_down"], f32)[0],
        "ident": np.eye(128, dtype=f32),
    }
    mk = np.zeros((128, 8, 512), f32)
    for kb in range(8):
        half = kb // 4
        keyp = kb * 128 + np.arange(128)[:, None]
        qp = half * 512 + np.arange(512)[None, :]
        mk[:, kb, :] = np.where(qp >= keyp, 0.0, NEGBIG).astype(f32)
    sh["maskt"] = mk.reshape(128, 8 * 512)
    maps = []
    for j in range(NCORES):
        own = slice(j * SO, (j + 1) * SO)
        xt = np.empty((D, S + HALO), f32)
        xt[:, 0:SO] = xT_full[:, own]
        xt[:, SO:SO + j * SO] = xT_full[:, 0:j * SO]
        xt[:, SO + j * SO:S] = xT_full[:, (j + 1) * SO:]
        if j > 0:
            xt[:, S:] = xT_full[:, j * SO - HALO:j * SO]
        else:
            xt[:, S:] = 0.0
        v01 = np.zeros((NH, S), f32)
        v01[:, 0:SO + j * SO] = 1.0
        vb = np.zeros((NH, S), f32)
        vb[:, SO + j * SO:] = NEGBIG
        m = dict(sh)
        m["xT"] = xt
        m["xown"] = np.ascontiguousarray(x[own])
        m["vis01"] = v01
        m["visb"] = vb
        m["hv"] = np.full((128, 1), 1.0 if j > 0 else 0.0, f32)
        maps.append(m)
    return maps


_CACHE = {}


def kernel(**inputs):
    if "nc" not in _CACHE:
        _CACHE["nc"] = build()[0]
    nc = _CACHE["nc"]
    maps = make_in_maps(inputs)
    res = run_bass_kernel_spmd(nc, maps, core_ids=list(range(NCORES)))
    outs = [np.asarray(res.results[j]["out"], np.float32) for j in range(NCORES)]
    return np.concatenate(outs, axis=0).reshape(1, S, D)
```

```python
import numpy as np
from contextlib import ExitStack
import concourse.bass as bass
import concourse.mybir as mybir
from concourse.bass_utils import run_bass_kernel_spmd

F32 = mybir.dt.float32
BF16 = mybir.dt.bfloat16
AF = mybir.ActivationFunctionType
ALU = mybir.AluOpType
AX = mybir.AxisListType

D = 4096
KC = 32
S = 8192
SO = 1024
HALO = 32
NH = 16
NE = 64
FF = 512
NCORES = 8
ALPHA = 2.0 ** 0.25
SCALE = 128.0 ** -0.5
EPS = 1e-5
NEGBIG = -30000.0
ND = 48


class Prog:
    def __init__(self, nc, es):
        self.nc = nc
        self.q = {k: [] for k in ("pe", "act", "dve", "pool", "sp")}
        self.sem = {}
        self.semobj = {}
        for e in ("pe", "act", "dve", "pool"):
            h = es.enter_context(nc.semaphore("s_" + e))
            self.sem[e] = h
            self.semobj[e] = h
        self.cnt = {e: 0 for e in self.sem}
        self.waited = {e: {} for e in self.q}
        self.lastw = {}
        self.readers = {}
        self.dpool = []
        for i in range(ND):
            h = es.enter_context(nc.semaphore("d%d" % i))
            self.dpool.append(h)
            self.semobj[("d", i)] = h
        self.dcount = 0
        self.dlast = {}
        self.ninstr = 0

    def _deps(self, reads, writes):
        deps = {}

        def add(k, v):
            if v > deps.get(k, 0):
                deps[k] = v

        for b in reads:
            w = self.lastw.get(b)
            if w:
                add(*w)
        for b in writes:
            w = self.lastw.get(b)
            if w:
                add(*w)
            r = self.readers.get(b)
            if r:
                for k, v in r.items():
                    add(k, v)
        return deps

    def _waits(self, eng, deps, skip=None):
        wd = self.waited[eng]
        for k, v in deps.items():
            if k == skip:
                continue
            if wd.get(k, 0) >= v:
                continue
            wd[k] = v
            h = self.semobj[k]
            self.q[eng].append(lambda e, h=h, v=v: e.wait_ge(h, v))

    def _record(self, tok, reads, writes):
        k, v = tok
        for b in reads:
            r = self.readers.setdefault(b, {})
            if v > r.get(k, 0):
                r[k] = v
        for b in writes:
            self.lastw[b] = tok
            self.readers[b] = {}

    def op(self, eng, fn, reads=(), writes=()):
        deps = self._deps(reads, writes)
        self._waits(eng, deps, skip=("pe" if eng == "pe" else None))
        self.cnt[eng] += 1
        n = self.cnt[eng]
        h = self.sem[eng]
        self.q[eng].append(lambda e, fn=fn, h=h: fn(e).then_inc(h, 1))
        self._record((eng, n), reads, writes)
        self.ninstr += 1

    def dma(self, queue, fn, reads=(), writes=()):
        deps = self._deps(reads, writes)
        i = self.dcount
        self.dcount += 1
        slot = i % ND
        use = i // ND
        k = ("d", slot)
        if use > 0:
            deps[k] = max(deps.get(k, 0), 16 * use)
        self._waits(queue, deps)
        h = self.dpool[slot]
        self.q[queue].append(lambda e, fn=fn, h=h: fn(e).then_inc(h, 16))
        self._record((k, 16 * (use + 1)), reads, writes)
        self.dlast[k] = 16 * (use + 1)
        self.ninstr += 1

    def barrier(self):
        deps = dict(self.dlast)
        for e in self.cnt:
            if self.cnt[e]:
                deps[e] = self.cnt[e]
        for qn in self.q:
            self._waits(qn, dict(deps))

    def finish(self):
        deps = dict(self.dlast)
        for e in self.cnt:
            if self.cnt[e]:
                deps[e] = self.cnt[e]
        self._waits("sp", deps)

    def replay(self):
        nc = self.nc
        q = self.q
        with nc.Block() as block:
            @block.tensor
            def _(e):
                for f in q["pe"]:
                    f(e)

            @block.scalar
            def _(e):
                for f in q["act"]:
                    f(e)

            @block.vector
            def _(e):
                for f in q["dve"]:
                    f(e)

            @block.gpsimd
            def _(e):
                for f in q["pool"]:
                    f(e)

            @block.sync
            def _(e):
                for f in q["sp"]:
                    f(e)


def build(upto=99, debug=()):
    nc = bass.Bass("TRN2", target_bir_lowering=False)
    es = ExitStack()

    declared = []

    def din(name, shape, dt=F32, need=0):
        if upto < need:
            return None
        declared.append(name)
        return nc.dram_tensor(name, list(shape), dt, kind="ExternalInput").ap()

    def dscr(name, shape, dt):
        kind = "ExternalOutput" if name in debug else "Internal"
        return nc.dram_tensor(name, list(shape), dt, kind=kind).ap()

    xT = din("xT", [D, S + HALO])
    xown = din("xown", [SO, D])
    c_l = din("c_l", [128, KC])
    w_ada = din("w_ada", [D, 6 * D])
    b_ada = din("b_ada", [1, 6 * D])
    w_in = din("w_in", [D, 10256])
    bfor = din("bfor", [NH, 1])
    wdw_l = din("wdw_l", [128, 16 * 31])
    bdw_l = din("bdw_l", [128, 16])
    cng_l = din("cng_l", [128, 16])
    cnb_l = din("cnb_l", [128, 16])
    w_out = din("w_out", [D, D], need=4)
    ln1_g = din("ln1_g", [1, D])
    ln1_b = din("ln1_b", [1, D])
    ln2_g = din("ln2_g", [1, D])
    ln2_b = din("ln2_b", [1, D])
    w_router = din("w_router", [D, NE])
    rbias = din("rbias", [1, NE])
    w_eg = din("w_eg", [NE, D, FF], need=6)
    w_eu = din("w_eu", [NE, D, FF], need=6)
    w_ed = din("w_ed", [NE, FF, D], need=7)
    w_sg = din("w_sg", [D, FF], need=6)
    w_su = din("w_su", [D, FF], need=6)
    w_sd = din("w_sd", [FF, D], need=7)
    vis01 = din("vis01", [NH, S])
    visb = din("visb", [NH, S])
    hv = din("hv", [128, 1])
    ident_in = din("ident", [128, 128])
    maskt_in = din("maskt", [128, 8 * 512])
    out = nc.dram_tensor("out", [SO, D], F32, kind="ExternalOutput").ap()

    modd = dscr("modd", [1, 6 * D], F32)
    hT = dscr("hT", [D, S + HALO], BF16)
    QT = dscr("QT", [NH * 128, SO], BF16)
    featT = dscr("featT", [D, SO], BF16)
    y1 = dscr("y1", [SO, D], F32)
    x1 = dscr("x1", [SO, D], F32)
    hidd = dscr("hidd", [65 * 4 * 128, SO], BF16)
    dbgG = dscr("dbgG", [SO, NE], F32)

    P = Prog(nc, es)

    def sb(st, name, shape, dt):
        return st.enter_context(nc.sbuf_tensor(name, list(shape), dt))

    def ps(st, name, shape, dt=F32):
        return st.enter_context(nc.psum_tensor(name, list(shape), dt))

    identf = sb(es, "identf", [128, 128], F32)
    identb = sb(es, "identb", [128, 128], BF16)
    onesb = sb(es, "onesb", [128, 128], BF16)
    onesf = sb(es, "onesf", [128, 128], F32)
    P.dma("sp", lambda e: e.dma_start(out=identf[:], in_=ident_in[:, :]), writes=["identf"])
    P.dma("pool", lambda e: e.dma_start(out=identb[:], in_=ident_in[:, :]), writes=["identb"])
    P.op("dve", lambda e: e.memset(onesb[:], 1.0), writes=["onesb"])
    P.op("dve", lambda e: e.memset(onesf[:], 1.0), writes=["onesf"])

    shsc = sb(es, "shsc", [128, 64], F32)
    nbAll = sb(es, "nbAll", [128, 64 * NH], F32)
    CQ = sb(es, "CQ", [NH, SO], F32)

    P.barrier()
    if upto >= 0:
        with ExitStack() as st:
            csb = sb(st, "csb", [128, KC], F32)
            scb = sb(st, "scb", [128, KC], BF16)
            wt = [sb(st, "wt%d" % i, [128, KC, 512], BF16) for i in range(3)]
            brow = [sb(st, "brow%d" % i, [1, 512], F32) for i in range(2)]
            mrow = [sb(st, "mrow%d" % i, [1, 512], F32) for i in range(2)]
            pm = [ps(st, "pm%d" % i, [1, 512]) for i in range(2)]
            wav = w_ada.rearrange("(kc p) n -> p kc n", p=128)
            P.dma("sp", lambda e: e.dma_start(out=csb[:], in_=c_l[:, :]), writes=["csb"])
            P.op("act", lambda e: e.activation(out=scb[:], in_=csb[:], func=AF.Silu), reads=["csb"], writes=["scb"])
            for ct in range(48):
                sl = ct % 3
                b2 = ct % 2
                P.dma("pool", lambda e, sl=sl, ct=ct: e.dma_start(out=wt[sl][:], in_=wav[:, :, ct * 512:(ct + 1) * 512]),
                      writes=["wt%d" % sl])
                P.dma("sp", lambda e, b2=b2, ct=ct: e.dma_start(out=brow[b2][:], in_=b_ada[0:1, ct * 512:(ct + 1) * 512]),
                      writes=["brow%d" % b2])
                for kc in range(KC):
                    P.op("pe", lambda e, b2=b2, sl=sl, kc=kc: e.matmul(pm[b2][:], lhsT=scb[:, kc:kc + 1], rhs=wt[sl][:, kc, :],
                                                                      start=(kc == 0), stop=(kc == KC - 1)),
                         reads=["wt%d" % sl, "scb"], writes=["pm%d" % b2])
                P.op("dve", lambda e, b2=b2: e.tensor_tensor(out=mrow[b2][:], in0=pm[b2][:], in1=brow[b2][:], op=ALU.add),
                     reads=["pm%d" % b2, "brow%d" % b2], writes=["mrow%d" % b2])
                P.dma("sp", lambda e, b2=b2, ct=ct: e.dma_start(out=modd[0:1, ct * 512:(ct + 1) * 512], in_=mrow[b2][:]),
                      reads=["mrow%d" % b2], writes=["modd"])
            mr = sb(st, "mr", [64, 128], F32)
            pt0 = ps(st, "pt0", [128, 512])
            P.dma("sp", lambda e: e.dma_start(out=mr[:], in_=modd[0, 0:2 * D].rearrange("(r p) -> r p", p=128)),
                  reads=["modd"], writes=["mr"])
            P.op("pe", lambda e: e.transpose(out=pt0[:, 0:64], in_=mr[:], identity=identf[0:64, 0:64]),
                 reads=["mr", "identf"], writes=["pt0"])
            P.op("dve", lambda e: e.tensor_copy(out=shsc[:, 0:32], in_=pt0[:, 0:32]), reads=["pt0"], writes=["shsc"])
            P.op("dve", lambda e: e.tensor_scalar(out=shsc[:, 32:64], in0=pt0[:, 32:64], scalar1=1.0, scalar2=None, op0=ALU.add),
                 reads=["pt0"], writes=["shsc"])

    P.barrier()
    stE = ExitStack()
    EL = sb(stE, "EL", [NH, S], F32)
    if upto >= 1:
        with ExitStack() as st:
            xs = [sb(st, "xs%d" % i, [128, KC, 256], F32) for i in range(2)]
            hb = [sb(st, "hb%d" % i, [128, KC, 256], BF16) for i in range(2)]
            wf = sb(st, "wf", [128, KC, NH], BF16)
            bf_sb = sb(st, "bf_sb", [NH, 1], F32)
            negb = sb(st, "negb", [NH, 1], F32)
            pfl = [ps(st, "pfl%d" % i, [NH, 512]) for i in range(2)]
            xTv = xT.rearrange("(kc p) t -> p kc t", p=128)
            hTv = hT.rearrange("(kc p) t -> p kc t", p=128)
            wiv = w_in.rearrange("(kc p) n -> p kc n", p=128)
            P.dma("pool", lambda e: e.dma_start(out=wf[:], in_=wiv[:, :, 6144:6160]), writes=["wf"])
            P.dma("sp", lambda e: e.dma_start(out=bf_sb[:], in_=bfor[:, :]), writes=["bf_sb"])
            P.op("dve", lambda e: e.tensor_scalar(out=negb[:], in0=bf_sb[:], scalar1=-1.0, scalar2=None, op0=ALU.mult),
                 reads=["bf_sb"], writes=["negb"])
            for t in range(33):
                w = 256 if t < 32 else HALO
                c0 = t * 256
                b2 = t % 2
                P.dma("sp", lambda e, b2=b2, c0=c0, w=w: e.dma_start(out=xs[b2][:, :, 0:w], in_=xTv[:, :, c0:c0 + w]),
                      writes=["xs%d" % b2])
                for kc in range(KC):
                    if kc % 2 == 0:
                        P.op("act", lambda e, b2=b2, kc=kc, w=w: e.activation(
                            out=hb[b2][:, kc, 0:w], in_=xs[b2][:, kc, 0:w], func=AF.Identity,
                            scale=shsc[:, 32 + kc:33 + kc], bias=shsc[:, kc:kc + 1]),
                            reads=["xs%d" % b2, "shsc"], writes=[("hb", b2, kc)])
                    else:
                        P.op("dve", lambda e, b2=b2, kc=kc, w=w: e.tensor_scalar(
                            out=hb[b2][:, kc, 0:w], in0=xs[b2][:, kc, 0:w],
                            scalar1=shsc[:, 32 + kc:33 + kc], scalar2=shsc[:, kc:kc + 1], op0=ALU.mult, op1=ALU.add),
                            reads=["xs%d" % b2, "shsc"], writes=[("hb", b2, kc)])
                P.dma("sp", lambda e, b2=b2, c0=c0, w=w: e.dma_start(out=hTv[:, :, c0:c0 + w], in_=hb[b2][:, :, 0:w]),
                      reads=[("hb", b2, kc) for kc in range(KC)], writes=[("hT", t)])
                if t < 32:
                    for kc in range(KC):
                        P.op("pe", lambda e, b2=b2, kc=kc: e.matmul(pfl[b2][:, 0:256], lhsT=wf[:, kc, :], rhs=hb[b2][:, kc, :],
                                                                  start=(kc == 0), stop=(kc == KC - 1)),
                             reads=["wf", ("hb", b2, kc)], writes=["pfl%d" % b2])
                    P.op("act", lambda e, b2=b2, c0=c0: e.activation(out=EL[:, c0:c0 + 256], in_=pfl[b2][:, 0:256], func=AF.Exp,
                                                                      scale=-1.0, bias=negb[:, 0:1]),
                         reads=["pfl%d" % b2, "negb"], writes=["EL"])
        P.barrier()
        with ExitStack() as st:
            PL = sb(st, "PL", [NH, S], F32)
            tmp = sb(st, "tmpv", [NH, S], F32)
            visx = sb(st, "visx", [NH, S], F32)
            ones16 = sb(st, "ones16", [NH, 2048], F32)
            TL = sb(st, "TL", [NH, 1], F32)
            pnb = ps(st, "pnb", [128, 64 * NH])
            P.op("dve", lambda e: e.memset(ones16[:], 1.0), writes=["ones16"])
            P.op("act", lambda e: e.activation(out=EL[:], in_=EL[:], func=AF.Ln, bias=1.0, scale=1.0), reads=["EL"], writes=["EL"])
            for ch in range(4):
                if ch == 0:
                    P.op("dve", lambda e: e.tensor_tensor_scan(out=PL[:, 0:2048], data0=ones16[:], data1=EL[:, 0:2048],
                                                                initial=0.0, op0=ALU.mult, op1=ALU.add),
                         reads=["EL", "ones16"], writes=["PL"])
                else:
                    P.op("dve", lambda e, ch=ch: e.tensor_tensor_scan(out=PL[:, ch * 2048:(ch + 1) * 2048], data0=ones16[:],
                                                                       data1=EL[:, ch * 2048:(ch + 1) * 2048],
                                                                       initial=PL[:, ch * 2048 - 1:ch * 2048], op0=ALU.mult, op1=ALU.add),
                         reads=["EL", "ones16", "PL"], writes=["PL"])
            P.dma("sp", lambda e: e.dma_start(out=visx[:], in_=vis01[:, :]), writes=["visx"])
            P.op("dve", lambda e: e.tensor_tensor(out=tmp[:], in0=EL[:], in1=visx[:], op=ALU.mult), reads=["EL", "visx"], writes=["tmpv"])
            P.op("dve", lambda e: e.reduce_sum(out=TL[:], in_=tmp[:], axis=AX.X), reads=["tmpv"], writes=["TL"])
            P.op("dve", lambda e: e.tensor_scalar(out=CQ[:], in0=PL[:, 0:SO], scalar1=TL[:, 0:1], scalar2=-1.0, op0=ALU.add, op1=ALU.mult),
                 reads=["PL", "TL"], writes=["CQ"])
            P.dma("sp", lambda e: e.dma_start(out=visx[:], in_=visb[:, :]), reads=[], writes=["visx"])
            P.op("dve", lambda e: e.tensor_tensor(out=tmp[:], in0=PL[:], in1=visx[:], op=ALU.add), reads=["PL", "visx"], writes=["tmpv"])
            P.op("dve", lambda e: e.tensor_scalar(out=tmp[:, 0:SO], in0=tmp[:, 0:SO], scalar1=TL[:, 0:1], scalar2=None, op0=ALU.add),
                 reads=["tmpv", "TL"], writes=["tmpv"])
            for kb in range(64):
                P.op("pe", lambda e, kb=kb: e.transpose(out=pnb[:, kb * NH:(kb + 1) * NH], in_=tmp[:, kb * 128:(kb + 1) * 128],
                                                        identity=identf[0:NH, 0:NH]),
                     reads=["tmpv", "identf"], writes=["pnb"])
            P.op("dve", lambda e: e.tensor_copy(out=nbAll[:], in_=pnb[:]), reads=["pnb"], writes=["nbAll"])

    stE.close()
    P.barrier()
    if upto >= 2:
        with ExitStack() as st:
            hown = sb(st, "hown", [128, KC, HALO + SO], BF16)
            wc = [sb(st, "wc%d" % i, [128, KC, 128], BF16) for i in range(3)]
            qsb = [sb(st, "qsb%d" % i, [128, SO], BF16) for i in range(2)]
            wdw = sb(st, "wdw", [128, 16 * 31], F32)
            bdw = sb(st, "bdw", [128, 16], F32)
            cng = sb(st, "cng", [128, 16], F32)
            cnb = sb(st, "cnb", [128, 16], F32)
            hvs = sb(st, "hvs", [128, 1], F32)
            onesdiv = sb(st, "onesdiv", [128, 128], F32)
            sg = sb(st, "sg", [128, HALO + SO], F32)
            glu = sb(st, "glu", [128, HALO + SO], F32)
            cacc_t = sb(st, "cacc", [128, SO], F32)
            usq = sb(st, "usq", [128, SO], F32)
            msb = sb(st, "msb", [128, 512], F32)
            var = sb(st, "var", [128, 512], F32)
            cen = sb(st, "cen", [128, 512], F32)
            fsb = [sb(st, "fsb%d" % i, [128, SO], BF16) for i in range(2)]
            P0 = ps(st, "P0", [128, 2048])
            P1 = ps(st, "P1", [128, 512])
            P2 = ps(st, "P2", [128, 512])
            P3 = ps(st, "P3", [128, 512])
            hTv = hT.rearrange("(kc p) t -> p kc t", p=128)
            wiv = w_in.rearrange("(kc p) n -> p kc n", p=128)
            P.dma("sp", lambda e: e.dma_start(out=hown[:, :, HALO:HALO + SO], in_=hTv[:, :, 0:SO]), reads=[("hT", t_) for t_ in range(33)], writes=["hown"])
            P.dma("sp", lambda e: e.dma_start(out=hown[:, :, 0:HALO], in_=hTv[:, :, S:S + HALO]), reads=[("hT", t_) for t_ in range(33)], writes=["hown"])
            P.dma("sp", lambda e: e.dma_start(out=wdw[:], in_=wdw_l[:, :]), writes=["wdw"])
            P.dma("sp", lambda e: e.dma_start(out=bdw[:], in_=bdw_l[:, :]), writes=["bdw"])
            P.dma("sp", lambda e: e.dma_start(out=cng[:], in_=cng_l[:, :]), writes=["cng"])
            P.dma("sp", lambda e: e.dma_start(out=cnb[:], in_=cnb_l[:, :]), writes=["cnb"])
            P.dma("sp", lambda e: e.dma_start(out=hvs[:], in_=hv[:, :]), writes=["hvs"])
            P.op("dve", lambda e: e.memset(onesdiv[:], 1.0 / 128.0), writes=["onesdiv"])
            nload = [0]

            def load_w(col0):
                sl = nload[0] % 3
                nload[0] += 1
                P.dma("pool", lambda e, sl=sl, col0=col0: e.dma_start(out=wc[sl][:], in_=wiv[:, :, col0:col0 + 128]),
                      writes=["wc%d" % sl])
                return sl

            if "dbg_hown" in debug:
                dbg_hown = dscr("dbg_hown", [128, KC * (HALO + SO)], BF16)
                P.dma("sp", lambda e: e.dma_start(out=dbg_hown[:, :], in_=hown[:].rearrange("p k t -> p (k t)")), reads=["hown"], writes=["dbg_hown"])
            for h in range(NH):
                sl = load_w(h * 128)
                if "dbg_wc" in debug and h == 0:
                    dbg_wc = dscr("dbg_wc", [128, KC * 128], BF16)
                    P.dma("sp", lambda e, sl=sl: e.dma_start(out=dbg_wc[:, :], in_=wc[sl][:].rearrange("p k t -> p (k t)")), reads=["wc%d" % sl], writes=["dbg_wc"])
                half = h % 2
                for nt in range(2):
                    for kc in range(KC):
                        P.op("pe", lambda e, sl=sl, kc=kc, nt=nt, half=half: e.matmul(
                            P0[:, half * 1024 + nt * 512: half * 1024 + (nt + 1) * 512], lhsT=wc[sl][:, kc, :],
                            rhs=hown[:, kc, HALO + nt * 512: HALO + (nt + 1) * 512], start=(kc == 0), stop=(kc == KC - 1)),
                            reads=["wc%d" % sl, "hown"], writes=[("P0", half, nt)])
                eng = "act" if h % 2 == 0 else "dve"
                if eng == "act":
                    P.op("act", lambda e, half=half: e.copy(out=qsb[half][:], in_=P0[:, half * 1024:(half + 1) * 1024]),
                         reads=[("P0", half, 0), ("P0", half, 1)], writes=["qsb%d" % half])
                else:
                    P.op("dve", lambda e, half=half: e.tensor_copy(out=qsb[half][:], in_=P0[:, half * 1024:(half + 1) * 1024]),
                         reads=[("P0", half, 0), ("P0", half, 1)], writes=["qsb%d" % half])
                P.dma("sp", lambda e, half=half, h=h: e.dma_start(out=QT[h * 128:(h + 1) * 128, :], in_=qsb[half][:]),
                      reads=["qsb%d" % half], writes=["QT"])
            for c in range(16):
                slv = load_w(6160 + c * 128)
                slg = load_w(8208 + c * 128)
                for half, sl in ((0, slv), (1, slg)):
                    for nt in range(2):
                        for kc in range(KC):
                            P.op("pe", lambda e, sl=sl, kc=kc, nt=nt, half=half: e.matmul(
                                P0[:, half * 1024 + nt * 512: half * 1024 + (nt + 1) * 512], lhsT=wc[sl][:, kc, :],
                                rhs=hown[:, kc, HALO + nt * 512: HALO + (nt + 1) * 512], start=(kc == 0), stop=(kc == KC - 1)),
                                reads=["wc%d" % sl, "hown"], writes=[("P0", half, nt)])
                    for kc in range(KC):
                        P.op("pe", lambda e, sl=sl, kc=kc, half=half: e.matmul(
                            P1[:, half * HALO:(half + 1) * HALO], lhsT=wc[sl][:, kc, :], rhs=hown[:, kc, 0:HALO],
                            start=(kc == 0), stop=(kc == KC - 1)),
                            reads=["wc%d" % sl, "hown"], writes=[("P1", half)])
                P.op("act", lambda e: e.activation(out=sg[:, HALO:HALO + SO], in_=P0[:, 1024:2048], func=AF.Sigmoid),
                     reads=[("P0", 1, 0), ("P0", 1, 1)], writes=["sg"])
                P.op("act", lambda e: e.activation(out=sg[:, 0:HALO], in_=P1[:, HALO:2 * HALO], func=AF.Sigmoid),
                     reads=[("P1", 1)], writes=["sgh"])
                P.op("dve", lambda e: e.tensor_tensor(out=glu[:, HALO:HALO + SO], in0=P0[:, 0:1024], in1=sg[:, HALO:HALO + SO], op=ALU.mult),
                     reads=[("P0", 0, 0), ("P0", 0, 1), "sg"], writes=["glu"])
                P.op("dve", lambda e: e.scalar_tensor_tensor(out=glu[:, 0:HALO], in0=P1[:, 0:HALO], scalar=hvs[:, 0:1], in1=sg[:, 0:HALO],
                                                             op0=ALU.mult, op1=ALU.mult),
                     reads=[("P1", 0), "sgh", "hvs"], writes=["gluh"])
                P.op("dve", lambda e, c=c: e.tensor_scalar(out=cacc_t[:], in0=glu[:, 2:2 + SO], scalar1=wdw[:, c * 31:c * 31 + 1],
                                                          scalar2=bdw[:, c:c + 1], op0=ALU.mult, op1=ALU.add),
                     reads=["glu", "gluh", "wdw", "bdw"], writes=["cacc"])
                for k in range(1, 31):
                    P.op("dve", lambda e, c=c, k=k: e.scalar_tensor_tensor(out=cacc_t[:], in0=glu[:, 2 + k:2 + k + SO],
                                                                          scalar=wdw[:, c * 31 + k:c * 31 + k + 1], in1=cacc_t[:],
                                                                          op0=ALU.mult, op1=ALU.add),
                         reads=["glu", "gluh", "wdw", "cacc"], writes=["cacc"])
                P.op("act", lambda e: e.activation(out=usq[:], in_=cacc_t[:], func=AF.Square), reads=["cacc"], writes=["usq"])
                f2 = c % 2
                for hf in range(2):
                    P.op("pe", lambda e, hf=hf: e.matmul(P2[:], lhsT=onesdiv[:], rhs=cacc_t[:, hf * 512:(hf + 1) * 512], start=True, stop=True),
                         reads=["onesdiv", "cacc"], writes=["P2"])
                    P.op("pe", lambda e, hf=hf: e.matmul(P3[:], lhsT=onesdiv[:], rhs=usq[:, hf * 512:(hf + 1) * 512], start=True, stop=True),
                         reads=["onesdiv", "usq"], writes=["P3"])
                    P.op("act", lambda e: e.copy(out=msb[:], in_=P2[:]), reads=["P2"], writes=["msb"])
                    P.op("dve", lambda e: e.tensor_tensor(out=var[:], in0=msb[:], in1=msb[:], op=ALU.mult), reads=["msb"], writes=["var"])
                    P.op("dve", lambda e: e.tensor_tensor(out=var[:], in0=P3[:], in1=var[:], op=ALU.subtract), reads=["P3", "var"], writes=["var"])
                    P.op("dve", lambda e: e.tensor_scalar(out=var[:], in0=var[:], scalar1=EPS, scalar2=None, op0=ALU.add), reads=["var"], writes=["var"])
                    P.op("act", lambda e: e.activation(out=var[:], in_=var[:], func=AF.Sqrt), reads=["var"], writes=["var"])
                    P.op("dve", lambda e: e.reciprocal(out=var[:], in_=var[:]), reads=["var"], writes=["var"])
                    P.op("dve", lambda e, hf=hf: e.tensor_tensor(out=cen[:], in0=cacc_t[:, hf * 512:(hf + 1) * 512], in1=msb[:], op=ALU.subtract),
                         reads=["cacc", "msb"], writes=["cen"])
                    P.op("dve", lambda e: e.tensor_tensor(out=cen[:], in0=cen[:], in1=var[:], op=ALU.mult), reads=["cen", "var"], writes=["cen"])
                    P.op("dve", lambda e, c=c: e.tensor_scalar(out=cen[:], in0=cen[:], scalar1=cng[:, c:c + 1], scalar2=cnb[:, c:c + 1],
                                                              op0=ALU.mult, op1=ALU.add), reads=["cen", "cng", "cnb"], writes=["cen"])
                    P.op("act", lambda e, hf=hf, f2=f2: e.activation(out=fsb[f2][:, hf * 512:(hf + 1) * 512], in_=cen[:], func=AF.Silu),
                         reads=["cen"], writes=["fsb%d" % f2])
                P.dma("sp", lambda e, c=c, f2=f2: e.dma_start(out=featT[2048 + c * 128: 2048 + (c + 1) * 128, :], in_=fsb[f2][:]),
                      reads=["fsb%d" % f2], writes=["featT"])

    P.barrier()
    if upto >= 3:
        with ExitStack() as st:
            wk = [sb(st, "wk%d" % i, [128, KC, 128], BF16) for i in range(2)]
            wv = [sb(st, "wv%d" % i, [128, KC, 128], BF16) for i in range(2)]
            ht = [sb(st, "ht%d" % i, [128, KC, 512], BF16) for i in range(3)]
            KT = sb(st, "KT", [128, S], BF16)
            V = sb(st, "V", [128, S], BF16)
            QTh = sb(st, "QTh", [128, SO], BF16)
            CQB = sb(st, "CQB", [128, SO], F32)
            sel = sb(st, "sel", [NH, 128], F32)
            X = [sb(st, "X%d" % i, [128, 512], F32) for i in range(4)]
            PT = [sb(st, "PT%d" % i, [128, 512], BF16) for i in range(4)]
            maskt = sb(st, "maskt_sb", [128, 8 * 512], BF16)
            rden = sb(st, "rden", [128, 512], F32)
            osb = [sb(st, "osb%d" % i, [128, 512], BF16) for i in range(2)]
            pk = [ps(st, "pk%d" % i, [128, 512]) for i in range(1)]
            pv = [ps(st, "pv%d" % i, [128, 512]) for i in range(1)]
            pS = [ps(st, "pS%d" % i, [128, 512]) for i in range(4)]
            pO = ps(st, "pO", [128, 512])
            pD = ps(st, "pD", [128, 512])
            hTv = hT.rearrange("(kc p) t -> p kc t", p=128)
            wiv = w_in.rearrange("(kc p) n -> p kc n", p=128)
            P.dma("pool", lambda e: e.dma_start(out=maskt[:], in_=maskt_in[:, :], max_dma_last_dim=2048 * 4), writes=["maskt"])
            ucount = 0
            nheads = NH if upto > 3 or True else 1
            for h in range(nheads):
                w2 = h % 2
                P.dma("pool", lambda e, w2=w2, h=h: e.dma_start(out=wk[w2][:], in_=wiv[:, :, 2048 + h * 128: 2048 + (h + 1) * 128]),
                      writes=["wk%d" % w2])
                P.dma("pool", lambda e, w2=w2, h=h: e.dma_start(out=wv[w2][:], in_=wiv[:, :, 4096 + h * 128: 4096 + (h + 1) * 128]),
                      writes=["wv%d" % w2])
                P.dma("sp", lambda e, h=h: e.dma_start(out=QTh[:], in_=QT[h * 128:(h + 1) * 128, :]), reads=["QT"], writes=["QTh"])
                P.op("dve", lambda e, h=h: e.tensor_scalar(out=sel[:], in0=onesf[0:NH, :], scalar1=identf[0:NH, h:h + 1], scalar2=None, op0=ALU.mult),
                     reads=["onesf", "identf"], writes=["sel"])
                for hf in range(2):
                    P.op("pe", lambda e, hf=hf: e.matmul(pS[hf][:], lhsT=sel[:], rhs=CQ[:, hf * 512:(hf + 1) * 512], start=True, stop=True),
                         reads=["sel", "CQ"], writes=["pS%d" % hf])
                    P.op("act", lambda e, hf=hf: e.copy(out=CQB[:, hf * 512:(hf + 1) * 512], in_=pS[hf][:]),
                         reads=["pS%d" % hf], writes=[("CQB", hf)])
                for tt in range(16):
                    b2 = tt % 3
                    P.dma("sp", lambda e, b2=b2, tt=tt: e.dma_start(out=ht[b2][:], in_=hTv[:, :, tt * 512:(tt + 1) * 512]),
                          reads=[("hT", t_) for t_ in range(33)], writes=["ht%d" % b2])
                    for kc in range(KC):
                        P.op("pe", lambda e, b2=b2, kc=kc, w2=w2: e.matmul(pk[0][:], lhsT=wk[w2][:, kc, :], rhs=ht[b2][:, kc, :],
                                                                          start=(kc == 0), stop=(kc == KC - 1)),
                             reads=["wk%d" % w2, "ht%d" % b2], writes=["pk0"])
                    P.op("act", lambda e, b2=b2, tt=tt: e.copy(out=KT[:, tt * 512:(tt + 1) * 512], in_=pk[0][:]),
                         reads=["pk0"], writes=[("KT", tt)])
                    for j in range(4):
                        for kc in range(KC):
                            P.op("pe", lambda e, b2=b2, kc=kc, w2=w2, j=j: e.matmul(pv[0][:, j * 128:(j + 1) * 128],
                                                                                   lhsT=ht[b2][:, kc, j * 128:(j + 1) * 128], rhs=wv[w2][:, kc, :],
                                                                                   start=(kc == 0), stop=(kc == KC - 1)),
                                 reads=["wv%d" % w2, "ht%d" % b2], writes=["pv0"])
                    P.op("dve", lambda e, b2=b2, tt=tt: e.tensor_copy(out=V[:, tt * 512:(tt + 1) * 512], in_=pv[0][:]),
                         reads=["pv0"], writes=[("V", tt)])
                for half in range(2):
                    kbs = [kb for kb in range(8) if kb // 4 <= half] + list(range(8, 64))
                    units = []
                    for idx, kb in enumerate(kbs):
                        units.append((ucount, idx, kb))
                        ucount += 1

                    def emit_S(u, idx, kb, half=half):
                        P.op("pe", lambda e, u=u, kb=kb, half=half: e.matmul(pS[u % 4][:], lhsT=KT[:, kb * 128:(kb + 1) * 128],
                                                                            rhs=QTh[:, half * 512:(half + 1) * 512], start=True, stop=True),
                             reads=[("KT", kb // 4), "QTh"], writes=["pS%d" % (u % 4)])

                    def emit_rest(u, idx, kb, half=half, h=h, n=len(kbs)):
                        P.op("dve", lambda e, u=u, half=half: e.scalar_tensor_tensor(out=X[u % 4][:], in0=pS[u % 4][:], scalar=SCALE,
                                                                                   in1=CQB[:, half * 512:(half + 1) * 512], op0=ALU.mult, op1=ALU.add),
                             reads=["pS%d" % (u % 4), ("CQB", half)], writes=["X%d" % (u % 4)])
                        if kb < 8 and kb // 4 == half:
                            P.op("pool", lambda e, u=u, kb=kb: e.tensor_tensor(out=X[u % 4][:], in0=X[u % 4][:], in1=maskt[:, kb * 512:(kb + 1) * 512], op=ALU.add),
                                 reads=["X%d" % (u % 4), "maskt"], writes=["X%d" % (u % 4)])
                        P.op("act", lambda e, u=u, kb=kb, h=h: e.activation(out=PT[u % 4][:], in_=X[u % 4][:], func=AF.Exp,
                                                                          bias=nbAll[:, kb * NH + h: kb * NH + h + 1], scale=1.0),
                             reads=["X%d" % (u % 4), "nbAll"], writes=["PT%d" % (u % 4)])
                        P.op("pe", lambda e, u=u, kb=kb, idx=idx, n=n: e.matmul(pO[:], lhsT=V[:, kb * 128:(kb + 1) * 128], rhs=PT[u % 4][:],
                                                                               start=(idx == 0), stop=(idx == n - 1)),
                             reads=[("V", kb // 4), "PT%d" % (u % 4)], writes=["pO"])
                        P.op("pe", lambda e, u=u, idx=idx, n=n: e.matmul(pD[:], lhsT=onesb[:], rhs=PT[u % 4][:],
                                                                        start=(idx == 0), stop=(idx == n - 1)),
                             reads=["onesb", "PT%d" % (u % 4)], writes=["pD"])

                    for i in range(3):
                        emit_S(*units[i])
                    for i, un in enumerate(units):
                        if i + 3 < len(units):
                            emit_S(*units[i + 3])
                        emit_rest(*un)
                    o2 = (h * 2 + half) % 2
                    P.op("dve", lambda e: e.reciprocal(out=rden[:], in_=pD[:]), reads=["pD"], writes=["rden"])
                    P.op("dve", lambda e, o2=o2: e.tensor_tensor(out=osb[o2][:], in0=pO[:], in1=rden[:], op=ALU.mult),
                         reads=["pO", "rden"], writes=["osb%d" % o2])
                    P.dma("sp", lambda e, o2=o2, h=h, half=half: e.dma_start(out=featT[h * 128:(h + 1) * 128, half * 512:(half + 1) * 512], in_=osb[o2][:]),
                          reads=["osb%d" % o2], writes=["featT"])

    P.barrier()
    if upto >= 4:
        with ExitStack() as st:
            Fm = sb(st, "Fm", [128, KC, SO], BF16)
            gab = sb(st, "gab", [128, D], F32)
            wo = [sb(st, "wo%d" % i, [128, KC, 512], BF16) for i in range(2)]
            xt = [sb(st, "xt%d" % i, [128, 512], F32) for i in range(3)]
            ysb = [sb(st, "ysb%d" % i, [128, 512], F32) for i in range(3)]
            py = [ps(st, "py%d" % i, [128, 512]) for i in range(4)]
            fTv = featT.rearrange("(kc p) t -> p kc t", p=128)
            wov = w_out.rearrange("(kc p) n -> p kc n", p=128)
            for q4 in range(4):
                P.dma("sp", lambda e, q4=q4: e.dma_start(out=Fm[:, q4 * 8:(q4 + 1) * 8, :], in_=fTv[:, q4 * 8:(q4 + 1) * 8, :]),
                      reads=["featT"], writes=["Fm"])
            P.dma("sp", lambda e: e.dma_start(out=gab[:], in_=modd[0, 2 * D:3 * D].partition_broadcast(128)), reads=["modd"], writes=["gab"])
            it = 0
            for dt in range(8):
                w2 = dt % 2
                P.dma("pool", lambda e, w2=w2, dt=dt: e.dma_start(out=wo[w2][:], in_=wov[:, :, dt * 512:(dt + 1) * 512]), writes=["wo%d" % w2])
                for tt in range(8):
                    x3 = it % 3
                    p4 = it % 4
                    it += 1
                    P.dma("sp", lambda e, x3=x3, tt=tt, dt=dt: e.dma_start(out=xt[x3][:], in_=xown[tt * 128:(tt + 1) * 128, dt * 512:(dt + 1) * 512]),
                          writes=["xt%d" % x3])
                    for kc in range(KC):
                        P.op("pe", lambda e, p4=p4, kc=kc, tt=tt, w2=w2: e.matmul(py[p4][:], lhsT=Fm[:, kc, tt * 128:(tt + 1) * 128], rhs=wo[w2][:, kc, :],
                                                                                 start=(kc == 0), stop=(kc == KC - 1)),
                             reads=["Fm", "wo%d" % w2], writes=["py%d" % p4])
                    P.op("dve", lambda e, p4=p4, x3=x3, dt=dt: e.tensor_tensor(out=ysb[x3][:], in0=py[p4][:], in1=gab[:, dt * 512:(dt + 1) * 512], op=ALU.mult),
                         reads=["py%d" % p4, "gab"], writes=["ysb%d" % x3])
                    P.op("dve", lambda e, x3=x3: e.scalar_tensor_tensor(out=ysb[x3][:], in0=xt[x3][:], scalar=ALPHA, in1=ysb[x3][:], op0=ALU.mult, op1=ALU.add),
                         reads=["xt%d" % x3, "ysb%d" % x3], writes=["ysb%d" % x3])
                    P.dma("sp", lambda e, x3=x3, tt=tt, dt=dt: e.dma_start(out=y1[tt * 128:(tt + 1) * 128, dt * 512:(dt + 1) * 512], in_=ysb[x3][:]),
                          reads=["ysb%d" % x3], writes=["y1"])

    P.barrier()
    stM = ExitStack()
    h2T = sb(stM, "h2T", [128, KC, SO], BF16)
    GT = sb(stM, "GT", [NE, SO], F32)

    def layer_norm_tile(st_tiles, yt, key, gkey, bkey, gb, bb):
        stats, mv, rstd = st_tiles
        for c8 in range(8):
            P.op("dve", lambda e, c8=c8: e.bn_stats(out=stats[:, c8 * 6:(c8 + 1) * 6], in_=yt[:, c8 * 512:(c8 + 1) * 512]),
                 reads=[key], writes=["stats"])
        P.op("dve", lambda e: e.bn_aggr(out=mv[:], in_=stats[:]), reads=["stats"], writes=["mv"])
        P.op("dve", lambda e: e.tensor_scalar(out=rstd[:], in0=mv[:, 1:2], scalar1=EPS, scalar2=None, op0=ALU.add), reads=["mv"], writes=["rstd"])
        P.op("act", lambda e: e.activation(out=rstd[:], in_=rstd[:], func=AF.Sqrt), reads=["rstd"], writes=["rstd"])
        P.op("dve", lambda e: e.reciprocal(out=rstd[:], in_=rstd[:]), reads=["rstd"], writes=["rstd"])
        P.op("dve", lambda e: e.tensor_scalar(out=yt[:], in0=yt[:], scalar1=mv[:, 0:1], scalar2=rstd[:, 0:1], op0=ALU.subtract, op1=ALU.mult),
             reads=[key, "mv", "rstd"], writes=[key])
        P.op("dve", lambda e: e.tensor_tensor(out=yt[:], in0=yt[:], in1=gb[:], op=ALU.mult), reads=[key, gkey], writes=[key])
        P.op("pool", lambda e: e.tensor_tensor(out=yt[:], in0=yt[:], in1=bb[:], op=ALU.add), reads=[key, bkey], writes=[key])

    if upto >= 5:
        with ExitStack() as st:
            g1b = sb(st, "g1b", [128, D], F32)
            b1b = sb(st, "b1b", [128, D], F32)
            scf = sb(st, "scf", [128, D], F32)
            shf = sb(st, "shf", [128, D], F32)
            yt = [sb(st, "yt%d" % i, [128, D], F32) for i in range(2)]
            h2t = [sb(st, "h2t%d" % i, [128, D], F32) for i in range(2)]
            stats = sb(st, "stats", [128, 48], F32)
            mv = sb(st, "mv", [128, 2], F32)
            rstd = sb(st, "rstd", [128, 1], F32)
            ptr = [ps(st, "ptr%d" % i, [128, 512]) for i in range(4)]
            P.dma("sp", lambda e: e.dma_start(out=g1b[:], in_=ln1_g[0, :].partition_broadcast(128)), writes=["g1b"])
            P.dma("sp", lambda e: e.dma_start(out=b1b[:], in_=ln1_b[0, :].partition_broadcast(128)), writes=["b1b"])
            P.dma("sp", lambda e: e.dma_start(out=scf[:], in_=modd[0, 4 * D:5 * D].partition_broadcast(128)), reads=["modd"], writes=["scf"])
            P.dma("sp", lambda e: e.dma_start(out=shf[:], in_=modd[0, 3 * D:4 * D].partition_broadcast(128)), reads=["modd"], writes=["shf"])
            P.op("pool", lambda e: e.tensor_scalar(out=scf[:], in0=scf[:], scalar1=1.0, scalar2=None, op0=ALU.add), reads=["scf"], writes=["scf"])
            npt = 0
            for tt in range(8):
                b2 = tt % 2
                P.dma("sp", lambda e, b2=b2, tt=tt: e.dma_start(out=yt[b2][:], in_=y1[tt * 128:(tt + 1) * 128, :]), reads=["y1"], writes=["yt%d" % b2])
                layer_norm_tile((stats, mv, rstd), yt[b2], "yt%d" % b2, "g1b", "b1b", g1b, b1b)
                P.dma("sp", lambda e, b2=b2, tt=tt: e.dma_start(out=x1[tt * 128:(tt + 1) * 128, :], in_=yt[b2][:]), reads=["yt%d" % b2], writes=["x1"])
                P.op("dve", lambda e, b2=b2: e.tensor_tensor(out=h2t[b2][:], in0=yt[b2][:], in1=scf[:], op=ALU.mult),
                     reads=["yt%d" % b2, "scf"], writes=["h2t%d" % b2])
                P.op("pool", lambda e, b2=b2: e.tensor_tensor(out=h2t[b2][:], in0=h2t[b2][:], in1=shf[:], op=ALU.add),
                     reads=["h2t%d" % b2, "shf"], writes=["h2t%d" % b2])
                for g4 in range(8):
                    p4 = npt % 4
                    npt += 1
                    for j in range(4):
                        kc = g4 * 4 + j
                        P.op("pe", lambda e, p4=p4, j=j, kc=kc, b2=b2: e.transpose(out=ptr[p4][:, j * 128:(j + 1) * 128], in_=h2t[b2][:, kc * 128:(kc + 1) * 128],
                                                                                identity=identf[:]),
                             reads=["h2t%d" % b2, "identf"], writes=["ptr%d" % p4])
                    dst = h2T[:, g4 * 4:(g4 + 1) * 4, tt * 128:(tt + 1) * 128]
                    src = ptr[p4][:].rearrange("p (j t) -> p j t", j=4)
                    if g4 % 2 == 0:
                        P.op("act", lambda e, dst=dst, src=src: e.copy(out=dst, in_=src), reads=["ptr%d" % p4], writes=[("h2T", tt)])
                    else:
                        P.op("dve", lambda e, dst=dst, src=src: e.tensor_copy(out=dst, in_=src), reads=["ptr%d" % p4], writes=[("h2T", tt)])
        P.barrier()
        with ExitStack() as st:
            wr = sb(st, "wr", [128, KC, NE], BF16)
            rbb = sb(st, "rbb", [128, NE], F32)
            sc = sb(st, "rsc", [128, NE], F32)
            bi = sb(st, "rbi", [128, NE], F32)
            m88 = sb(st, "m88", [128, 64], F32)
            gs = sb(st, "gs", [128, 8], F32)
            m8 = sb(st, "m8", [128, 8], F32)
            gm = sb(st, "gm", [128, 8], F32)
            gn = sb(st, "gn", [128, 8], F32)
            msk = sb(st, "msk", [128, NE], F32)
            selr = sb(st, "selr", [128, NE], F32)
            ssum = sb(st, "ssum", [128, 1], F32)
            G = sb(st, "G", [128, 8 * NE], F32)
            pr = [ps(st, "pr%d" % i, [128, 512]) for i in range(2)]
            pgt = ps(st, "pgt", [NE, SO])
            P.dma("pool", lambda e: e.dma_start(out=wr[:], in_=w_router.rearrange("(kc p) n -> p kc n", p=128)), writes=["wr"])
            P.dma("sp", lambda e: e.dma_start(out=rbb[:], in_=rbias[0, :].partition_broadcast(128)), writes=["rbb"])
            for tt in range(8):
                b2 = tt % 2
                for kc in range(KC):
                    P.op("pe", lambda e, b2=b2, kc=kc, tt=tt: e.matmul(pr[b2][:, 0:NE], lhsT=h2T[:, kc, tt * 128:(tt + 1) * 128], rhs=wr[:, kc, :],
                                                                      start=(kc == 0), stop=(kc == KC - 1)),
                         reads=[("h2T", tt), "wr"], writes=["pr%d" % b2])
                P.op("act", lambda e, b2=b2: e.activation(out=sc[:], in_=pr[b2][:, 0:NE], func=AF.Sigmoid), reads=["pr%d" % b2], writes=["rsc"])
                P.op("dve", lambda e: e.tensor_tensor(out=bi[:], in0=sc[:], in1=rbb[:], op=ALU.add), reads=["rsc", "rbb"], writes=["rbi"])
                for g in range(8):
                    P.op("dve", lambda e, g=g: e.max(out=m88[:, g * 8:(g + 1) * 8], in_=bi[:, g * 8:(g + 1) * 8]), reads=["rbi"], writes=["m88"])
                m3 = m88[:].rearrange("p (g k) -> p g k", k=8)
                P.op("dve", lambda e, m3=m3: e.tensor_tensor(out=gs[:], in0=m3[:, :, 0], in1=m3[:, :, 1], op=ALU.add), reads=["m88"], writes=["gs"])
                P.op("dve", lambda e: e.max(out=m8[:], in_=gs[:]), reads=["gs"], writes=["m8"])
                P.op("dve", lambda e: e.tensor_scalar(out=gm[:], in0=gs[:], scalar1=m8[:, 3:4], scalar2=None, op0=ALU.is_ge), reads=["gs", "m8"], writes=["gm"])
                P.op("dve", lambda e: e.tensor_scalar(out=gn[:], in0=gm[:], scalar1=-1.0, scalar2=-NEGBIG, op0=ALU.add, op1=ALU.mult), reads=["gm"], writes=["gn"])
                for g in range(8):
                    P.op("dve", lambda e, g=g: e.tensor_scalar(out=msk[:, g * 8:(g + 1) * 8], in0=bi[:, g * 8:(g + 1) * 8], scalar1=gm[:, g:g + 1],
                                                              scalar2=gn[:, g:g + 1], op0=ALU.mult, op1=ALU.add), reads=["rbi", "gm", "gn"], writes=["msk"])
                P.op("dve", lambda e: e.max(out=m8[:], in_=msk[:]), reads=["msk"], writes=["m8"])
                P.op("dve", lambda e: e.tensor_scalar(out=selr[:], in0=msk[:], scalar1=m8[:, 5:6], scalar2=None, op0=ALU.is_ge), reads=["msk", "m8"], writes=["selr"])
                P.op("dve", lambda e: e.tensor_tensor(out=selr[:], in0=selr[:], in1=sc[:], op=ALU.mult), reads=["selr", "rsc"], writes=["selr"])
                P.op("dve", lambda e: e.reduce_sum(out=ssum[:], in_=selr[:], axis=AX.X), reads=["selr"], writes=["ssum"])
                P.op("dve", lambda e: e.reciprocal(out=ssum[:], in_=ssum[:]), reads=["ssum"], writes=["ssum"])
                P.op("dve", lambda e, tt=tt: e.tensor_scalar(out=G[:, tt * NE:(tt + 1) * NE], in0=selr[:], scalar1=ssum[:, 0:1], scalar2=2.5, op0=ALU.mult, op1=ALU.mult),
                     reads=["selr", "ssum"], writes=[("G", tt)])
                if "dbgG" in debug:
                    P.dma("sp", lambda e, tt=tt: e.dma_start(out=dbgG[tt * 128:(tt + 1) * 128, :], in_=G[:, tt * NE:(tt + 1) * NE]), reads=[("G", tt)], writes=["dbgG"])
                P.op("pe", lambda e, tt=tt: e.transpose(out=pgt[:, tt * 128:(tt + 1) * 128], in_=G[:, tt * NE:(tt + 1) * NE], identity=identf[:]),
                     reads=[("G", tt), "identf"], writes=["pgt"])
            P.op("dve", lambda e: e.tensor_copy(out=GT[:], in_=pgt[:]), reads=["pgt"], writes=["GT"])

    P.barrier()
    if upto >= 6:
        with ExitStack() as st:
            wsl = [sb(st, "wsl%d" % i, [128, 16, 512], BF16) for i in range(6)]
            GB = [sb(st, "GB%d" % i, [128, SO], F32) for i in range(2)]
            sele = sb(st, "sele", [NE, 128], F32)
            sgt = [sb(st, "sgt%d" % i, [128, 512], F32) for i in range(2)]
            hh = [sb(st, "hh%d" % i, [128, 512], F32) for i in range(2)]
            hidG = [sb(st, "hidG%d" % i, [128, SO], BF16) for i in range(2)]
            pg = [ps(st, "pg%d" % i, [128, 512]) for i in range(3)]
            pu = [ps(st, "pu%d" % i, [128, 512]) for i in range(3)]
            pG = [ps(st, "pG%d" % i, [128, 512]) for i in range(2)]
            stages = [(ex, pr2) for ex in range(65) for pr2 in range(2)]

            def wviews(ex):
                if ex < NE:
                    return (w_eg[ex].rearrange("(kc p) f -> p kc f", p=128), w_eu[ex].rearrange("(kc p) f -> p kc f", p=128))
                return (w_sg.rearrange("(kc p) f -> p kc f", p=128), w_su.rearrange("(kc p) f -> p kc f", p=128))

            emitted = [0]

            def ensure(qmax):
                while emitted[0] <= qmax and emitted[0] < 65 * 4:
                    q = emitted[0]
                    emitted[0] += 1
                    ex, j = divmod(q, 4)
                    src = wviews(ex)[j // 2][:, (j % 2) * 16:(j % 2 + 1) * 16, :]
                    sl = q % 6
                    P.dma("pool", lambda e, sl=sl, src=src: e.dma_start(out=wsl[sl][:], in_=src), writes=["wsl%d" % sl])

            ensure(5)
            nhalf = 0
            for si, (ex, pr2) in enumerate(stages):
                if pr2 == 0:
                    ensure(4 * ex + 5)
                g2 = ex % 2
                if ex < NE and pr2 == 0:
                    P.op("dve", lambda e, ex=ex: e.tensor_scalar(out=sele[:], in0=onesf[0:NE, :], scalar1=identf[0:NE, ex:ex + 1], scalar2=None, op0=ALU.mult),
                         reads=["onesf", "identf"], writes=["sele"])
                    for hf in range(2):
                        P.op("pe", lambda e, hf=hf: e.matmul(pG[hf][:], lhsT=sele[:], rhs=GT[:, hf * 512:(hf + 1) * 512], start=True, stop=True),
                             reads=["sele", "GT"], writes=["pG%d" % hf])
                        P.op("act", lambda e, hf=hf, g2=g2: e.copy(out=GB[g2][:, hf * 512:(hf + 1) * 512], in_=pG[hf][:]),
                             reads=["pG%d" % hf], writes=[("GB", g2, hf)])
                for fl in range(2):
                    fc = pr2 * 2 + fl
                    u = ex * 4 + fc
                    w2 = u % 2
                    for hf in range(2):
                        p3 = nhalf % 3
                        s2 = nhalf % 2
                        nhalf += 1
                        for kc in range(KC):
                            P.op("pe", lambda e, p3=p3, ex=ex, fc=fc, kc=kc, hf=hf: e.matmul(pg[p3][:], lhsT=wsl[(4 * ex + kc // 16) % 6][:, kc % 16, fc * 128:(fc + 1) * 128], rhs=h2T[:, kc, hf * 512:(hf + 1) * 512],
                                                                                          start=(kc == 0), stop=(kc == KC - 1)),
                                 reads=["wsl%d" % ((4 * ex + kc // 16) % 6)] + [("h2T", t) for t in range(hf * 4, hf * 4 + 4)], writes=["pg%d" % p3])
                        if fc == 3 and hf == 1:
                            ensure(4 * ex + 7)
                        for kc in range(KC):
                            P.op("pe", lambda e, p3=p3, ex=ex, fc=fc, kc=kc, hf=hf: e.matmul(pu[p3][:], lhsT=wsl[(4 * ex + 2 + kc // 16) % 6][:, kc % 16, fc * 128:(fc + 1) * 128], rhs=h2T[:, kc, hf * 512:(hf + 1) * 512],
                                                                                          start=(kc == 0), stop=(kc == KC - 1)),
                                 reads=["wsl%d" % ((4 * ex + 2 + kc // 16) % 6)] + [("h2T", t) for t in range(hf * 4, hf * 4 + 4)], writes=["pu%d" % p3])
                        P.op("act", lambda e, p3=p3, s2=s2: e.activation(out=sgt[s2][:], in_=pg[p3][:], func=AF.Silu), reads=["pg%d" % p3], writes=["sgt%d" % s2])
                        if ex < NE:
                            P.op("dve", lambda e, p3=p3, s2=s2: e.tensor_tensor(out=hh[s2][:], in0=pu[p3][:], in1=sgt[s2][:], op=ALU.mult),
                                 reads=["pu%d" % p3, "sgt%d" % s2], writes=["hh%d" % s2])
                            P.op("dve", lambda e, s2=s2, w2=w2, hf=hf, g2=g2: e.tensor_tensor(out=hidG[w2][:, hf * 512:(hf + 1) * 512], in0=hh[s2][:],
                                                                                             in1=GB[g2][:, hf * 512:(hf + 1) * 512], op=ALU.mult),
                                 reads=["hh%d" % s2, ("GB", g2, hf)], writes=[("hidG", w2, hf)])
                        else:
                            P.op("dve", lambda e, p3=p3, s2=s2, w2=w2, hf=hf: e.tensor_tensor(out=hidG[w2][:, hf * 512:(hf + 1) * 512], in0=pu[p3][:], in1=sgt[s2][:], op=ALU.mult),
                                 reads=["pu%d" % p3, "sgt%d" % s2], writes=[("hidG", w2, hf)])
                    P.dma("sp", lambda e, w2=w2, u=u: e.dma_start(out=hidd[u * 128:(u + 1) * 128, :], in_=hidG[w2][:]),
                          reads=[("hidG", w2, 0), ("hidG", w2, 1)], writes=[("hidd", u)])

    stM.close()
    P.barrier()
    if upto >= 7:
        with ExitStack() as st:
            acc = sb(st, "acc", [128, 8 * D], F32)
            with ExitStack() as st2:
                wd = [sb(st2, "wd%d" % i, [128, 4, 2048], BF16) for i in range(3)]
                hin = [sb(st2, "hin%d" % i, [128, 4, SO], BF16) for i in range(2)]
                tmpa = [sb(st2, "tmpa%d" % i, [128, 512], F32) for i in range(2)]
                pd = [ps(st2, "pd%d" % i, [128, 512]) for i in range(4)]
                ngrp = 0
                steps = [(ex, dh) for ex in range(65) for dh in range(2)]

                def issue_wd(k):
                    ex, dh = steps[k]
                    w3 = k % 3
                    wdv = (w_ed[ex] if ex < NE else w_sd).rearrange("(f p) d -> p f d", p=128)
                    P.dma("pool", lambda e, w3=w3, wdv=wdv, dh=dh: e.dma_start(out=wd[w3][:], in_=wdv[:, :, dh * 2048:(dh + 1) * 2048]), writes=["wd%d" % w3])

                def issue_hin(ex):
                    h2 = ex % 2
                    P.dma("sp", lambda e, h2=h2, ex=ex: e.dma_start(out=hin[h2][:], in_=hidd[ex * 512:(ex + 1) * 512, :].rearrange("(f p) t -> p f t", p=128)),
                          reads=[("hidd", ex * 4 + f) for f in range(4)], writes=["hin%d" % h2])

                issue_hin(0)
                issue_wd(0)
                issue_wd(1)
                for k, (ex, dh) in enumerate(steps):
                    if k + 2 < len(steps):
                        issue_wd(k + 2)
                    if dh == 0 and ex + 1 < 65:
                        issue_hin(ex + 1)
                    h2 = ex % 2
                    w3 = k % 3
                    for tt in range(8):
                        for dq in range(4):
                            p4 = ngrp % 4
                            for f in range(4):
                                P.op("pe", lambda e, p4=p4, h2=h2, f=f, tt=tt, w3=w3, dq=dq: e.matmul(pd[p4][:], lhsT=hin[h2][:, f, tt * 128:(tt + 1) * 128],
                                                                                                   rhs=wd[w3][:, f, dq * 512:(dq + 1) * 512], start=(f == 0), stop=(f == 3)),
                                     reads=["hin%d" % h2, "wd%d" % w3], writes=["pd%d" % p4])
                            col = tt * D + dh * 2048 + dq * 512
                            akey = ("acc", tt, dh, dq)
                            if ex == 0:
                                P.op("act", lambda e, p4=p4, col=col: e.copy(out=acc[:, col:col + 512], in_=pd[p4][:]), reads=["pd%d" % p4], writes=[akey])
                            else:
                                P.op("dve", lambda e, p4=p4, col=col: e.tensor_tensor(out=acc[:, col:col + 512], in0=pd[p4][:], in1=acc[:, col:col + 512], op=ALU.add),
                                     reads=["pd%d" % p4, akey], writes=[akey])
                            ngrp += 1
            P.barrier()
            with ExitStack() as st2:
                gfb = sb(st2, "gfb", [128, D], F32)
                g2b = sb(st2, "g2b", [128, D], F32)
                b2b = sb(st2, "b2b", [128, D], F32)
                x1t = [sb(st2, "x1t%d" % i, [128, D], F32) for i in range(1)]
                stats = sb(st2, "stats2", [128, 48], F32)
                mv = sb(st2, "mv2", [128, 2], F32)
                rstd = sb(st2, "rstd2", [128, 1], F32)
                P.dma("sp", lambda e: e.dma_start(out=gfb[:], in_=modd[0, 5 * D:6 * D].partition_broadcast(128)), reads=["modd"], writes=["gfb"])
                P.dma("sp", lambda e: e.dma_start(out=g2b[:], in_=ln2_g[0, :].partition_broadcast(128)), writes=["g2b"])
                P.dma("sp", lambda e: e.dma_start(out=b2b[:], in_=ln2_b[0, :].partition_broadcast(128)), writes=["b2b"])
                for tt in range(8):
                    b2 = 0
                    at = acc[:, tt * D:(tt + 1) * D]
                    akeys = [("acc", tt, dh, dq) for dh in range(2) for dq in range(4)]
                    key = ("accrow", tt)
                    P.dma("sp", lambda e, b2=b2, tt=tt: e.dma_start(out=x1t[b2][:], in_=x1[tt * 128:(tt + 1) * 128, :]), reads=["x1"], writes=["x1t%d" % b2])
                    P.op("dve", lambda e, at=at: e.tensor_tensor(out=at, in0=at, in1=gfb[:], op=ALU.mult), reads=akeys + ["gfb"], writes=akeys + [key])
                    P.op("dve", lambda e, at=at, b2=b2: e.scalar_tensor_tensor(out=at, in0=x1t[b2][:], scalar=ALPHA, in1=at, op0=ALU.mult, op1=ALU.add),
                         reads=[key, "x1t%d" % b2], writes=[key])

                    class _T:
                        def __init__(self, ap):
                            self.ap = ap

                        def __getitem__(self, idx):
                            if idx == slice(None):
                                return self.ap
                            return self.ap[idx]
                    layer_norm_tile((stats, mv, rstd), _T(at), key, "g2b", "b2b", g2b, b2b)
                    P.dma("sp", lambda e, at=at, tt=tt: e.dma_start(out=out[tt * 128:(tt + 1) * 128, :], in_=at), reads=[key], writes=["out"])
    else:
        with ExitStack() as st:
            z = sb(st, "zt", [128, D], F32)
            P.op("dve", lambda e: e.memset(z[:], 0.0), writes=["zt"])
            for tt in range(8):
                P.dma("sp", lambda e, tt=tt: e.dma_start(out=out[tt * 128:(tt + 1) * 128, :], in_=z[:]), reads=["zt"], writes=["out"])

    P.finish()
    P.replay()
    es.close()
    P.declared = declared
    return nc, P


def make_in_maps(inputs):
    f32 = np.float32
    x = np.asarray(inputs["x"], f32)[0]
    xT_full = np.ascontiguousarray(x.T)
    sh = {
        "c_l": np.ascontiguousarray(np.asarray(inputs["c"], f32).reshape(KC, 128).T),
        "w_ada": np.asarray(inputs["w_ada"], f32)[0],
        "b_ada": np.asarray(inputs["b_ada"], f32).reshape(1, 6 * D),
        "w_in": np.asarray(inputs["w_in"], f32)[0],
        "bfor": np.asarray(inputs["b_forget"], f32).reshape(NH, 1),
        "wdw_l": np.ascontiguousarray(np.asarray(inputs["w_dw"], f32)[0].reshape(31, 16, 128).transpose(2, 1, 0).reshape(128, 16 * 31)),
        "bdw_l": np.ascontiguousarray(np.asarray(inputs["b_dw"], f32).reshape(16, 128).T),
        "cng_l": np.ascontiguousarray(np.asarray(inputs["conv_norm_g"], f32).reshape(16, 128).T),
        "cnb_l": np.ascontiguousarray(np.asarray(inputs["conv_norm_b"], f32).reshape(16, 128).T),
        "w_out": np.asarray(inputs["w_out"], f32)[0],
        "ln1_g": np.asarray(inputs["ln1_g"], f32).reshape(1, D),
        "ln1_b": np.asarray(inputs["ln1_b"], f32).reshape(1, D),
        "ln2_g": np.asarray(inputs["ln2_g"], f32).reshape(1, D),
        "ln2_b": np.asarray(inputs["ln2_b"], f32).reshape(1, D),
        "w_router": np.asarray(inputs["w_router"], f32)[0],
        "rbias": np.asarray(inputs["router_bias"], f32).reshape(1, NE),
        "w_eg": np.asarray(inputs["w_exp_gate"], f32)[0],
        "w_eu": np.asarray(inputs["w_exp_up"], f32)[0],
        "w_ed": np.asarray(inputs["w_exp_down"], f32)[0],
        "w_sg": np.asarray(inputs["w_sh_gate"], f32)[0],
        "w_su": np.asarray(inputs["w_sh_up"], f32)[0],
        "w_sd": np.asarray(inputs["w_sh_down"], f32)[0],
        "ident": np.eye(128, dtype=f32),
    }
    mk = np.zeros((128, 8, 512), f32)
    for kb in range(8):
        half = kb // 4
        keyp = kb * 128 + np.arange(128)[:, None]
        qp = half * 512 + np.arange(512)[None, :]
        mk[:, kb, :] = np.where(qp >= keyp, 0.0, NEGBIG).astype(f32)
    sh["maskt"] = mk.reshape(128, 8 * 512)
    maps = []
    for j in range(NCORES):
        own = slice(j * SO, (j + 1) * SO)
        xt = np.empty((D, S + HALO), f32)
        xt[:, 0:SO] = xT_full[:, own]
        xt[:, SO:SO + j * SO] = xT_full[:, 0:j * SO]
        xt[:, SO + j * SO:S] = xT_full[:, (j + 1) * SO:]
        if j > 0:
            xt[:, S:] = xT_full[:, j * SO - HALO:j * SO]
        else:
            xt[:, S:] = 0.0
        v01 = np.zeros((NH, S), f32)
        v01[:, 0:SO + j * SO] = 1.0
        vb = np.zeros((NH, S), f32)
        vb[:, SO + j * SO:] = NEGBIG
        m = dict(sh)
        m["xT"] = xt
        m["xown"] = np.ascontiguousarray(x[own])
        m["vis01"] = v01
        m["visb"] = vb
        m["hv"] = np.full((128, 1), 1.0 if j > 0 else 0.0, f32)
        maps.append(m)
    return maps


_CACHE = {}


def kernel(**inputs):
    if "nc" not in _CACHE:
        _CACHE["nc"] = build()[0]
    nc = _CACHE["nc"]
    maps = make_in_maps(inputs)
    res = run_bass_kernel_spmd(nc, maps, core_ids=list(range(NCORES)))
    outs = [np.asarray(res.results[j]["out"], np.float32) for j in range(NCORES)]
    return np.concatenate(outs, axis=0).reshape(1, S, D)
```
